# Optimizing a Trainium2 kernel written in Bass

```python
import math
import jax, jax.numpy as jnp
from jax import lax
import numpy as np

D_MODEL = 1024
BATCH = 4
SEQ = 4096
DEPTH = 2

CHUNK = 64
HEAD_DIM = 64
D_POOL = D_MODEL // 4
POOL_WINDOWS = (2, 4, 8, 16)
N_POOL_GROUPS = len(POOL_WINDOWS)
POOL_GROUP = D_POOL // N_POOL_GROUPS
D_CHUNK = 3 * D_MODEL // 8
N_CHUNK_HEADS = D_CHUNK // HEAD_DIM
LEFT_CHUNKS = 8
BAND = (LEFT_CHUNKS + 1) * CHUNK
REL_CLIP = 128
D_SB = D_MODEL - D_POOL - D_CHUNK
N_SB_HEADS = D_SB // HEAD_DIM
SB_BLOCK = 128
D_IN = D_POOL + 3 * D_CHUNK + 3 * D_SB
D_FF = ((8 * D_MODEL // 3 + 127) // 128) * 128
N_EXPERTS = 8
TOP_K = 2
D_FF_EXPERT = D_FF
N_DENSE = (DEPTH + 1) // 2
N_MOE = DEPTH // 2
ALPHA = (2.0 * DEPTH) ** 0.25
BETA = (8.0 * DEPTH) ** -0.25
LN_EPS = 1e-5
RMS_EPS = 1e-6
NEG_INF = -1e30

kernel_name = "hybrid_pool_chunkattn_stickbreak_moe_deepnorm"


def layer_norm(x, g, b):
    xf = x.astype(jnp.float32)
    mu = jnp.mean(xf, axis=-1, keepdims=True)
    xc = xf - mu
    var = jnp.mean(xc * xc, axis=-1, keepdims=True)
    y = xc * lax.rsqrt(var + LN_EPS) * g.astype(jnp.float32) + b.astype(jnp.float32)
    return y.astype(x.dtype)


def rms_normalise(y):
    yf = y.astype(jnp.float32)
    return yf * lax.rsqrt(jnp.mean(yf * yf, axis=-1, keepdims=True) + RMS_EPS)


def pool_mixer(u, pool_w, pool_scale):
    B, S, _ = u.shape
    uf = u.astype(jnp.float32)
    cs = jnp.cumsum(uf, axis=1)
    t = jnp.arange(S, dtype=jnp.float32)[None, :, None]
    outs = []
    for g, w in enumerate(POOL_WINDOWS):
        c = cs[..., g * POOL_GROUP:(g + 1) * POOL_GROUP]
        lag = jnp.pad(c[:, :-w], ((0, 0), (w, 0), (0, 0)))
        cnt = jnp.minimum(t + 1.0, float(w))
        outs.append((c - lag) / cnt)
    pooled = jnp.concatenate(outs, axis=-1) - uf
    pooled = pooled.reshape(B, S, N_POOL_GROUPS, POOL_GROUP)
    y = jnp.einsum('bsgc,gcd->bsgd', pooled, pool_w.astype(jnp.float32))
    return y.reshape(B, S, D_POOL) * pool_scale.astype(jnp.float32)


def chunk_attention(q, k, v, rel_bias):
    B, S, H, d = q.shape
    nc = S // CHUNK
    qc = q.reshape(B, nc, CHUNK, H, d).astype(jnp.float32)
    pad = ((0, 0), (LEFT_CHUNKS, 0), (0, 0), (0, 0), (0, 0))
    kc = jnp.pad(k.reshape(B, nc, CHUNK, H, d).astype(jnp.float32), pad)
    vc = jnp.pad(v.reshape(B, nc, CHUNK, H, d).astype(jnp.float32), pad)
    kband = jnp.concatenate([kc[:, j:j + nc] for j in range(LEFT_CHUNKS + 1)], axis=2)
    vband = jnp.concatenate([vc[:, j:j + nc] for j in range(LEFT_CHUNKS + 1)], axis=2)
    scores = jnp.einsum('bnqhd,bnkhd->bhnqk', qc, kband) * (1.0 / math.sqrt(d))
    qi = jnp.arange(CHUNK)[:, None]
    kj = jnp.arange(BAND)[None, :]
    rel = jnp.clip(qi + LEFT_CHUNKS * CHUNK - kj, -REL_CLIP, REL_CLIP) + REL_CLIP
    bias = rel_bias.astype(jnp.float32)[:, rel]
    key_chunk = jnp.arange(nc)[:, None] - LEFT_CHUNKS + jnp.arange(BAND)[None, :] // CHUNK
    valid = key_chunk >= 0
    scores = scores + bias[None, :, None, :, :]
    scores = jnp.where(valid[None, None, :, None, :], scores, NEG_INF)
    p = jax.nn.softmax(scores, axis=-1)
    o = jnp.einsum('bhnqk,bnkhd->bnqhd', p, vband)
    return o.reshape(B, S, H * d)


def stick_breaking_attention(q, k, v):
    B, S, H, d = q.shape
    qh = q.transpose(0, 2, 1, 3).astype(jnp.float32)
    kh = k.transpose(0, 2, 1, 3).astype(jnp.float32)
    vh = v.transpose(0, 2, 1, 3).astype(jnp.float32)
    scale = 1.0 / math.sqrt(d)
    outs = []
    for blk in range(S // SB_BLOCK):
        lo = blk * SB_BLOCK
        hi = lo + SB_BLOCK
        z = jnp.einsum('bhqd,bhkd->bhqk', qh[:, :, lo:hi], kh[:, :, :hi]) * scale
        t = lo + jnp.arange(SB_BLOCK)[:, None]
        s = jnp.arange(hi)[None, :]
        before = s < t
        log_keep = jnp.where(before, jax.nn.log_sigmoid(-z), 0.0)
        suffix = lax.cumsum(log_keep, axis=3, reverse=True) - log_keep
        w = jnp.where(before, jnp.exp(jax.nn.log_sigmoid(z) + suffix), 0.0)
        outs.append(jnp.einsum('bhqk,bhkd->bhqd', w, vh[:, :, :hi]))
    o = jnp.concatenate(outs, axis=2)
    return o.transpose(0, 2, 1, 3).reshape(B, S, H * d)


def hybrid_mixer(x, w_in, pool_w, pool_scale, rel_bias, g_mix, w_out):
    B, S, _ = x.shape
    h = x @ w_in
    u_pool = h[..., :D_POOL]
    qkv_c = h[..., D_POOL:D_POOL + 3 * D_CHUNK]
    qkv_s = h[..., D_POOL + 3 * D_CHUNK:]
    y_pool = pool_mixer(u_pool, pool_w, pool_scale)
    qc, kc, vc = [a.reshape(B, S, N_CHUNK_HEADS, HEAD_DIM) for a in jnp.split(qkv_c, 3, axis=-1)]
    y_chunk = chunk_attention(qc, kc, vc, rel_bias)
    qs, ks, vs = [a.reshape(B, S, N_SB_HEADS, HEAD_DIM) for a in jnp.split(qkv_s, 3, axis=-1)]
    y_sb = stick_breaking_attention(qs, ks, vs)
    y = jnp.concatenate([rms_normalise(y_pool), rms_normalise(y_chunk), rms_normalise(y_sb)], axis=-1)
    y = (y * g_mix.astype(jnp.float32)).astype(x.dtype)
    return y @ w_out


def swiglu(x, wg, wu, wd):
    return (jax.nn.silu(x @ wg) * (x @ wu)) @ wd


def moe_swiglu(x, router, wg, wu, wd):
    B, S, D = x.shape
    xt = x.reshape(B * S, D)
    logits = (xt @ router).astype(jnp.float32)
    top_val, top_idx = lax.top_k(logits, TOP_K)
    top_w = jax.nn.softmax(top_val, axis=-1)
    gates = jnp.sum(jax.nn.one_hot(top_idx, N_EXPERTS, dtype=jnp.float32) * top_w[..., None], axis=1)
    out = jnp.zeros((B * S, D), jnp.float32)
    for e in range(N_EXPERTS):
        out = out + gates[:, e:e + 1] * swiglu(xt, wg[e], wu[e], wd[e]).astype(jnp.float32)
    return out.reshape(B, S, D).astype(x.dtype)


def setup_inputs(seed: int = 0) -> dict:
    key = jax.random.key(seed)
    ks = jax.random.split(key, 20)
    f32 = jnp.float32
    n = lambda k, shape, s: jax.random.normal(k, shape, f32) * s
    x = jax.random.normal(ks[0], (BATCH, SEQ, D_MODEL), f32)
    w_in = n(ks[1], (DEPTH, D_MODEL, D_IN), D_MODEL ** -0.5)
    pool_w = n(ks[2], (DEPTH, N_POOL_GROUPS, POOL_GROUP, POOL_GROUP), POOL_GROUP ** -0.5)
    pool_scale = 1.0 + n(ks[3], (DEPTH, D_POOL), 0.05)
    rel_bias = n(ks[4], (DEPTH, N_CHUNK_HEADS, 2 * REL_CLIP + 1), 0.3)
    g_mix = 1.0 + n(ks[5], (DEPTH, D_MODEL), 0.05)
    w_out = n(ks[6], (DEPTH, D_MODEL, D_MODEL), BETA * D_MODEL ** -0.5)
    ln1_g = 1.0 + n(ks[7], (DEPTH, D_MODEL), 0.05)
    ln1_b = n(ks[8], (DEPTH, D_MODEL), 0.02)
    ln2_g = 1.0 + n(ks[9], (DEPTH, D_MODEL), 0.05)
    ln2_b = n(ks[10], (DEPTH, D_MODEL), 0.02)
    ffn_wg = n(ks[11], (N_DENSE, D_MODEL, D_FF), D_MODEL ** -0.5)
    ffn_wu = n(ks[12], (N_DENSE, D_MODEL, D_FF), D_MODEL ** -0.5)
    ffn_wd = n(ks[13], (N_DENSE, D_FF, D_MODEL), BETA * D_FF ** -0.5)
    moe_router = n(ks[14], (N_MOE, D_MODEL, N_EXPERTS), D_MODEL ** -0.5)
    moe_wg = n(ks[15], (N_MOE, N_EXPERTS, D_MODEL, D_FF_EXPERT), D_MODEL ** -0.5)
    moe_wu = n(ks[16], (N_MOE, N_EXPERTS, D_MODEL, D_FF_EXPERT), D_MODEL ** -0.5)
    moe_wd = n(ks[17], (N_MOE, N_EXPERTS, D_FF_EXPERT, D_MODEL), BETA * D_FF_EXPERT ** -0.5)
    return {"x": x, "w_in": w_in, "pool_w": pool_w, "pool_scale": pool_scale, "rel_bias": rel_bias,
            "g_mix": g_mix, "w_out": w_out, "ln1_g": ln1_g, "ln1_b": ln1_b, "ln2_g": ln2_g, "ln2_b": ln2_b,
            "ffn_wg": ffn_wg, "ffn_wu": ffn_wu, "ffn_wd": ffn_wd, "moe_router": moe_router,
            "moe_wg": moe_wg, "moe_wu": moe_wu, "moe_wd": moe_wd}


def reference(x, w_in, pool_w, pool_scale, rel_bias, g_mix, w_out, ln1_g, ln1_b, ln2_g, ln2_b,
              ffn_wg, ffn_wu, ffn_wd, moe_router, moe_wg, moe_wu, moe_wd):
    for i in range(DEPTH):
        m = hybrid_mixer(x, w_in[i], pool_w[i], pool_scale[i], rel_bias[i], g_mix[i], w_out[i])
        x = layer_norm(ALPHA * x + m, ln1_g[i], ln1_b[i])
        j = i // 2
        if i % 2 == 0:
            f = swiglu(x, ffn_wg[j], ffn_wu[j], ffn_wd[j])
        else:
            f = moe_swiglu(x, moe_router[j], moe_wg[j], moe_wu[j], moe_wd[j])
        x = layer_norm(ALPHA * x + f, ln2_g[i], ln2_b[i])
    return x
```

```python
import numpy as np
from contextlib import ExitStack
import concourse.bass as bass
import concourse.mybir as mybir
from concourse.bass_utils import run_bass_kernel_spmd

F32 = mybir.dt.float32
BF16 = mybir.dt.bfloat16
AF = mybir.ActivationFunctionType
ALU = mybir.AluOpType

ENGS = ("pe", "act", "dve", "pool", "sp")

D = 1024
SEQ = 4096
TT = 512
NT = 8
DFF = 2816
NE = 8
ALPHA = 4.0 ** 0.25
LN_EPS = 1e-5
RMS_EPS = 1e-6
OWN = (1, 3, 5, 7)
NFILL = 2

P_PSCALE = 0
P_GMIX = 2
P_LN1G = 10
P_LN1B = 18
P_LN2G = 26
P_LN2B = 34
P_BIASC = 42
P_CMASK = 48
P_ICNT = 50
P_GTAB = 82
P_M0 = 82 + 6 * 256
NPRM = P_M0 + 1
C_TRI = 0
C_TRIU = 128
C_ONES = 256
C_MASK = 384
C_IDENT = 512
C_NEG = 384
NCST = 640


class Buf:
    __slots__ = ("name", "w", "rd", "dsem", "dcnt", "dkey", "excl")

    def __init__(self, name="", excl=False):
        self.name = name
        self.excl = excl
        self.w = None
        self.rd = []
        self.dsem = None
        self.dcnt = 0
        self.dkey = None


class Sched:
    def __init__(self, nc, es):
        self.nc = nc
        self.es = es
        self.eng = dict(pe=nc.tensor, act=nc.scalar, dve=nc.vector, pool=nc.gpsimd, sp=nc.sync)
        self.semh = {}
        for e in ENGS:
            self.semh[e] = es.enter_context(nc.semaphore("s_" + e))
        self.cnt = {e: 0 for e in ENGS}
        self.pending = {e: False for e in ENGS}
        self.seen = {e: {} for e in ENGS}
        self.snap = {}
        self.nwait = 0
        self.nop = 0
        self.ndsem = 0
        self.dead = False
        self.dbufs = []

    def _wait(self, e, t):
        if t is None:
            return
        key, val = t
        if key == e and e == "pe":
            return
        sn = self.seen[e]
        if sn.get(key, 0) >= val:
            return
        if key == e and self.cnt[e] < val:
            raise RuntimeError("self-wait on pending ticket " + e)
        self.eng[e].wait_ge(self.semh[key], val)
        self.nwait += 1
        sn[key] = val
        s = self.snap.get(t)
        if s is not None:
            for k, v in s:
                if sn.get(k, 0) < v:
                    sn[k] = v

    def _deps(self, e, reads, writes):
        for b in reads:
            self._wait(e, b.w)
        for b in writes:
            self._wait(e, b.w)
            for t in b.rd:
                self._wait(e, t)

    def _commit(self, t, reads, writes):
        for b in reads:
            b.rd.append(t)
            if len(b.rd) > 64:
                last = {}
                for k, v in b.rd:
                    if last.get(k, 0) < v:
                        last[k] = v
                b.rd = list(last.items())
        for b in writes:
            b.w = t
            b.rd = []

    def op(self, e, fn, reads=(), writes=(), signal=True):
        if self.dead:
            return None
        ex = [b for b in reads if b.excl]
        if ex:
            writes = list(writes) + ex
        self._deps(e, reads, writes)
        ins = fn(self.eng[e])
        self.nop += 1
        if signal:
            ins.then_inc(self.semh[e], 1)
            self.cnt[e] += 1
            t = (e, self.cnt[e])
            self.pending[e] = False
            sn = self.seen[e]
            self.snap[t] = tuple((k, sn.get(k, 0)) for k in ENGS if k != e and sn.get(k, 0) > 0)
        else:
            t = (e, self.cnt[e] + 1)
            self.pending[e] = True
        self._commit(t, reads, writes)
        return t

    def dma(self, q, out, in_, sb, reads=(), writes=(), **kw):
        if self.dead:
            return None
        self._deps(q, reads, writes)
        if sb.dsem is None:
            sb.dsem = self.es.enter_context(self.nc.semaphore("d%d" % self.ndsem))
            sb.dkey = ("d", self.ndsem)
            self.semh[sb.dkey] = sb.dsem
            self.ndsem += 1
            self.dbufs.append(sb)
        ins = self.eng[q].dma_start(out=out, in_=in_, **kw)
        ins.then_inc(sb.dsem, 16)
        sb.dcnt += 16
        t = (sb.dkey, sb.dcnt)
        self._commit(t, reads, writes)
        return t

    def barrier(self):
        if self.dead:
            return
        ts = [(e, self.cnt[e]) for e in ENGS if self.cnt[e] > 0]
        for e in ENGS:
            for t in ts:
                if t[0] != e:
                    self._wait(e, t)

    def finish(self, bufs):
        for b in self.dbufs:
            self._wait("sp", (b.dkey, b.dcnt))
        for b in bufs:
            self._wait("sp", b.w)
            for t in b.rd:
                self._wait("sp", t)
        for e in ENGS:
            assert not self.pending[e], e


class TPool:
    def __init__(self, nc, es, name, shape, dtype, n):
        self.t = [es.enter_context(nc.sbuf_tensor("%s%d" % (name, i), shape, dtype)) for i in range(n)]
        self.b = [Buf("%s%d" % (name, i)) for i in range(n)]
        self.i = 0

    def get(self):
        i = self.i
        self.i = (i + 1) % len(self.t)
        return self.t[i], self.b[i]


class _Stop(Exception):
    pass


def build_fused(debug=False, stage=None):
    nc = bass.Bass("TRN2", target_bir_lowering=False)

    def chk(name):
        if stage == name:
            S.dead = True
    dt = nc.dram_tensor
    xT = dt("xT", [D, SEQ], F32, kind="ExternalInput").ap()
    w_in_all = dt("w_in", [2, D, 2560], F32, kind="ExternalInput").ap()
    w_out_all = dt("w_out", [2, D, D], F32, kind="ExternalInput").ap()
    pwbd_all = dt("pwbd", [2, 128, 2, 128], F32, kind="ExternalInput").ap()
    prm_all = dt("prm", [2, 128, NPRM], F32, kind="ExternalInput").ap()
    cst_d = dt("cst", [128, NCST], F32, kind="ExternalInput").ap()
    wg_d = dt("wg", [1, D * DFF], F32, kind="ExternalInput").ap()
    wu_d = dt("wu", [1, D * DFF], F32, kind="ExternalInput").ap()
    wd_d = dt("wd", [1, DFF * D], F32, kind="ExternalInput").ap()
    wg_m = dt("mwg", [NE, D * DFF], F32, kind="ExternalInput").ap()
    wu_m = dt("mwu", [NE, D * DFF], F32, kind="ExternalInput").ap()
    wd_m = dt("mwd", [NE, DFF * D], F32, kind="ExternalInput").ap()
    router = dt("router", [D, NE], F32, kind="ExternalInput").ap()
    outT = dt("outT", [D, 4 * TT], F32, kind="ExternalOutput").ap()
    x1s = dt("x1s", [D, SEQ], F32, kind="Internal").ap()
    x2s = dt("x2s", [D, SEQ], F32, kind="Internal").ap()
    xs2 = dt("xs2", [D, SEQ], F32, kind="Internal").ap()

    xT_v = xT.rearrange("(k p) n -> p k n", p=128)
    outT_v = outT.rearrange("(k p) n -> p k n", p=128)
    x1s_v = x1s.rearrange("(k p) n -> p k n", p=128)
    x2s_v = x2s.rearrange("(k p) n -> p k n", p=128)
    xs2_v = xs2.rearrange("(k p) n -> p k n", p=128)

    with ExitStack() as es0:
        S = Sched(nc, es0)
        ps = [es0.enter_context(nc.psum_tensor("ps%d" % i, [128, 512], F32)) for i in range(8)]
        Bps = [Buf("ps%d" % i, True) for i in range(8)]
        prm = es0.enter_context(nc.sbuf_tensor("prm_sb", [128, NPRM], F32))
        cstf = es0.enter_context(nc.sbuf_tensor("cstf", [128, 256], F32))
        cstb = es0.enter_context(nc.sbuf_tensor("cstb", [128, NCST], BF16))
        Bprm, Bcstf, Bcstb = Buf("prm"), Buf("cstf"), Buf("cstb")
        S.dma("sp", cstf[:, 0:128], cst_d[:, C_ONES:C_ONES + 128], Bcstf, writes=[Bcstf])
        S.dma("sp", cstf[:, 128:256], cst_d[:, C_IDENT:C_IDENT + 128], Bcstf, writes=[Bcstf])
        S.dma("pool", cstb[:], cst_d, Bcstb, writes=[Bcstb])
        Bout = [Buf("out%d" % i) for i in range(4)]
        onesf = cstf[:, 0:128]
        identf = cstf[:, 128:256]

        def pcol(c, n=1):
            return prm[:, c:c + n]

        def stats_rinv(srcs, scale, eps, fp, bank, Bbank):
            n = len(srcs)
            for i, (a, b) in enumerate(srcs):
                S.op("pe", lambda e: e.matmul(bank[:, :], lhsT=onesf, rhs=a, start=(i == 0), stop=(i == n - 1)),
                     [Bcstf, b], [Bbank])
            l, Bl = fp.get()
            S.op("act", lambda e: e.activation(out=l[:], in_=bank[:, :], func=AF.Ln, scale=scale, bias=eps), [Bbank], [Bl])
            r, Br = fp.get()
            S.op("act", lambda e: e.activation(out=r[:], in_=l[:], func=AF.Exp, scale=-0.5), [Bl], [Br])
            return r, Br

        def layer_norm(vch, Bv, gcol, bcol, fp, bankA, BbankA, bankB, BbankB, MR, BMR):
            sqs = []
            for d in range(8):
                S.op("pe", lambda e: e.matmul(bankA[:, :], lhsT=onesf, rhs=vch(d), start=(d == 0), stop=(d == 7)),
                     [Bcstf, Bv[d]], [BbankA])
            for d in range(8):
                sq, Bsq = fp.get()
                S.op("pool", lambda e: e.tensor_tensor(out=sq[:], in0=vch(d), in1=vch(d), op=ALU.mult), [Bv[d]], [Bsq])
                S.op("pe", lambda e: e.matmul(bankB[:, :], lhsT=onesf, rhs=sq[:], start=(d == 0), stop=(d == 7)),
                     [Bcstf, Bsq], [BbankB])
            M, BM = MR[0], BMR[0]
            S.op("act", lambda e: e.activation(out=M[:], in_=bankA[:, :], func=AF.Identity, scale=1.0 / D), [BbankA], [BM])
            msq, Bmsq = fp.get()
            S.op("dve", lambda e: e.tensor_tensor(out=msq[:], in0=M[:], in1=M[:], op=ALU.mult), [BM], [Bmsq])
            V, BV = fp.get()
            S.op("dve", lambda e: e.scalar_tensor_tensor(out=V[:], in0=bankB[:, :], scalar=1.0 / D, in1=msq[:],
                                                         op0=ALU.mult, op1=ALU.subtract), [BbankB, Bmsq], [BV])
            L, BL = fp.get()
            S.op("act", lambda e: e.activation(out=L[:], in_=V[:], func=AF.Ln, bias=LN_EPS), [BV], [BL])
            Rr, BR = MR[1], BMR[1]
            S.op("act", lambda e: e.activation(out=Rr[:], in_=L[:], func=AF.Exp, scale=-0.5), [BL], [BR])
            for d in range(8):
                t1, Bt1 = fp.get()
                S.op("pool", lambda e: e.tensor_tensor(out=t1[:], in0=vch(d), in1=M[:], op=ALU.subtract), [Bv[d], BM], [Bt1])
                S.op("dve", lambda e: e.tensor_tensor(out=t1[:], in0=t1[:], in1=Rr[:], op=ALU.mult), [Bt1, BR], [Bt1])
                S.op("act", lambda e: e.activation(out=vch(d), in_=t1[:], func=AF.Identity,
                                                   scale=pcol(gcol + d), bias=pcol(bcol + d)), [Bt1, Bprm], [Bv[d]])

        def emit_layer(li, moe, full, xsrc_v, Bxsrc, dst_v, Bdst):
            pfx = "L%d_" % li
            w_in_v = w_in_all[li].rearrange("(k p) f -> p k f", p=128)
            w_out_v = w_out_all[li].rearrange("(k p) f -> p k f", p=128)
            pwbd = pwbd_all[li]
            wg, wu, wd = (wg_m, wu_m, wd_m) if moe else (wg_d, wu_d, wd_d)
            S.dma("sp", prm[:], prm_all[li], Bprm, writes=[Bprm])
            Bx1s = [Buf("x1s%d" % i) for i in range(8)]
            with ExitStack() as es:
                sb = lambda name, shape, dtp: es.enter_context(nc.sbuf_tensor(pfx + name, shape, dtp))
                w_in_bf = sb("w_in_bf", [128, 8, 2560], BF16)
                w_out_bf = sb("w_out_bf", [128, 8, D], BF16)
                pw_bf = sb("pw_bf", [128, 2, 128], BF16)
                kT_s = sb("kT_s", [128, 3, SEQ], BF16)
                v_s = sb("v_s", [128, 32, 384], BF16)
                kT_c = sb("kT_c", [128, 3, 2 * TT], BF16)
                v_c = sb("v_c", [128, 8, 384], BF16)
                qT_c = sb("qT_c", [128, 3, TT], BF16)
                qT_s = sb("qT_s", [128, 3, TT], BF16)
                xbf = [sb("xbf0", [128, 8, TT], BF16)] * 2
                ubuf = [sb("ubuf%d" % i, [128, 2, 16 + TT], F32) for i in range(2)]
                ytile = [sb("ytile%d" % i, [128, TT], F32) for i in range(3)]
                Bytile = [Buf() for i in range(3)]
                lnMR = [sb("lnMR%d" % i, [128, TT], F32) for i in range(2)]
                BlnMR = [Buf(), Buf()]
                yn = sb("yn", [128, 8, TT], BF16)
                xres = sb("xres", [128, 8, TT], F32)
                small = sb("small", [128, 32], F32)
                fp = TPool(nc, es, pfx + "fp", [128, 512], F32, 5)
                zp = TPool(nc, es, pfx + "zp", [128, 16 + TT], F32, 4)
                bp = TPool(nc, es, pfx + "bp", [128, 512], BF16, 8)

                Bwin = [Buf("win%d" % i) for i in range(4)]
                Bwout, Bpw = Buf("wout"), Buf("pw")
                BkTs = [[Buf() for c in range(3)] for t in range(NT)]
                Bvs = [[Buf() for s in range(4)] for t in range(NT)]
                BkTc = [[Buf() for c in range(3)] for sl in range(2)]
                Bvc = [[Buf() for s in range(4)] for sl in range(2)]
                BqTc = [Buf() for c in range(3)]
                BqTs = [Buf() for c in range(3)]
                BqTn = [Buf() for c in range(3)]
                Bxbf = [Buf("xbf0")] * 2
                Bu = [Buf("u0"), Buf("u1")]
                Bpt = [Buf("pt0"), Buf("pt1")]
                Byn = [Buf() for c in range(8)]
                Bxres = [Buf() for c in range(8)]
                Bxres_d = Buf("xres_d")
                Bsmall = Buf("small")

                for c in range(4):
                    S.dma("pool", w_in_bf[:, :, c * 640:(c + 1) * 640], w_in_v[:, :, c * 640:(c + 1) * 640], Bwin[c], writes=[Bwin[c]])
                S.dma("pool", pw_bf[:], pwbd, Bpw, writes=[Bpw])
                S.op("dve", lambda e: e.tensor_scalar(out=small[:, 0:6], in0=pcol(P_BIASC, 6), scalar1=pcol(P_CMASK + 1),
                                                      scalar2=None, op0=ALU.add), [Bprm], [Bsmall])
                S.op("dve", lambda e: e.memset(ubuf[0][:, :, 0:16], 0.0), [], [Bu[0]])

                pj_i = [0]

                def pjbank():
                    i = pj_i[0]
                    pj_i[0] = 1 - i
                    return ps[i], Bps[i]

                ev_i = [0]

                def evac(out, in_, reads, writes, scale=None):
                    ev_i[0] ^= 1
                    if ev_i[0]:
                        if scale is None:
                            S.op("act", lambda e: e.activation(out=out, in_=in_, func=AF.Copy), reads, writes)
                        else:
                            S.op("act", lambda e: e.activation(out=out, in_=in_, func=AF.Identity, scale=scale), reads, writes)
                    else:
                        if scale is None:
                            S.op("dve", lambda e: e.tensor_copy(out=out, in_=in_), reads, writes)
                        else:
                            S.op("dve", lambda e: e.tensor_scalar(out=out, in0=in_, scalar1=scale, scalar2=None, op0=ALU.mult),
                                 reads, writes)

                Z = [ps[2], ps[3]]
                BZ = [Bps[2], Bps[3]]
                Abank = [ps[4], ps[5]]
                B4 = [Buf("ps4lo", True), Buf("ps4hi", True)]
                BA = [B4, [Bps[5]]]
                Obank = ps[6]
                BO = [Buf("O_lo", True), Buf("O_hi", True)]
                STb, BST = ps[7], Bps[7]
                BR2 = B4

                own_i = 0
                chk("setup")
                for t in range(NT):
                    if t == 1:
                        chk("proj0")
                    if t == 2:
                        chk("own1")
                    own = full or (t in OWN)
                    first_own = (t == (0 if full else 1))
                    sl = t % 2
                    xb, Bxb = xbf[sl], Bxbf[sl]
                    S.dma("pool", xb[:], xsrc_v[:, :, t * TT:(t + 1) * TT], Bxb, reads=[Bxsrc[t]], writes=[Bxb])
                    if t == 0:
                        S.dma("pool", w_out_bf[:], w_out_v, Bwout, writes=[Bwout])
                    if own:
                        S.dma("sp", xres[:], xsrc_v[:, :, t * TT:(t + 1) * TT], Bxres_d, reads=[Bxsrc[t]], writes=Bxres + [Bxres_d])

                    def proj_fm(col, out_fn):
                        pb, Bpb = pjbank()
                        for k in range(8):
                            S.op("pe", lambda e: e.matmul(pb[:, :], lhsT=w_in_bf[:, k, col:col + 128], rhs=xb[:, k, :],
                                                          start=(k == 0), stop=(k == 7)),
                                 [Bwin[col // 640], Bxb], [Bpb], signal=(k == 7))
                        out_fn(pb, Bpb)

                    if t == 1:
                        chk("t1a")
                    U = ubuf[sl]
                    for c in range(2):
                        proj_fm(c * 128, lambda pb, Bpb: evac(U[:, c, 16:16 + TT], pb[:, :], [Bpb], [Bu[sl]]))
                    if t < NT - 1:
                        S.op("dve", lambda e: e.tensor_copy(out=ubuf[1 - sl][:, :, 0:16], in_=U[:, :, TT:TT + 16]), [Bu[sl]], [Bu[1 - sl]])
                    for c in range(3):
                        proj_fm(640 + c * 128, lambda pb, Bpb: evac(kT_c[:, c, sl * TT:(sl + 1) * TT], pb[:, :], [Bpb], [BkTc[sl][c]]))
                    for c in range(3):
                        proj_fm(1792 + c * 128, lambda pb, Bpb: evac(kT_s[:, c, t * TT:(t + 1) * TT], pb[:, :], [Bpb], [BkTs[t][c]]))
                    if t == 1:
                        chk("t1b")
                    if own:
                        for c in range(3):
                            proj_fm(256 + c * 128, lambda pb, Bpb: evac(qT_c[:, c, :], pb[:, :], [Bpb], [BqTc[c]], scale=0.125))
                        for c in range(3):
                            proj_fm(1408 + c * 128, lambda pb, Bpb: evac(qT_s[:, c, :], pb[:, :], [Bpb], [BqTs[c]], scale=0.125))
                    if t == 1:
                        chk("t1c")
                    for s in range(4):
                        for (col, dst, Bd) in ((1024, v_c[:, sl * 4 + s, :], Bvc[sl][s]), (2176, v_s[:, t * 4 + s, :], Bvs[t][s])):
                            pb, Bpb = pjbank()
                            wb = sorted(set([col // 640, (col + 383) // 640]))
                            for k in range(8):
                                S.op("pe", lambda e: e.matmul(pb[:, 0:384], lhsT=xb[:, k, s * 128:(s + 1) * 128], rhs=w_in_bf[:, k, col:col + 384],
                                                              start=(k == 0), stop=(k == 7)),
                                     [Bwin[i] for i in wb] + [Bxb], [Bpb], signal=(k == 7))
                            evac(dst, pb[:, 0:384], [Bpb], [Bd])
                    if not own:
                        continue

                    chk("proj1")
                    W = 16 + TT
                    (TA, BTA), (TB, BTB) = zp.get(), zp.get()
                    Bpt = [BTA, BTB]
                    pooled = []
                    for c in range(2):
                        pl, Bpl = bp.get()
                        pooled.append((pl, Bpl))

                        def pool_out(p0, Tsrc, BT, inv):
                            S.op("dve", lambda e: e.scalar_tensor_tensor(out=pl[p0:p0 + 64, :], in0=Tsrc[p0:p0 + 64, 16:W], scalar=inv,
                                                                         in1=U[p0:p0 + 64, c, 16:W], op0=ALU.mult, op1=ALU.subtract),
                                 [BT, Bu[sl]], [Bpl])
                            if first_own:
                                tmp, Btmp = fp.get()
                                S.op("dve", lambda e: e.tensor_tensor(out=tmp[p0:p0 + 64, 0:16], in0=Tsrc[p0:p0 + 64, 16:32],
                                                                      in1=prm[p0:p0 + 64, P_ICNT + c * 16:P_ICNT + c * 16 + 16], op=ALU.mult),
                                     [BT, Bprm], [Btmp])
                                S.op("dve", lambda e: e.tensor_tensor(out=pl[p0:p0 + 64, 0:16], in0=tmp[p0:p0 + 64, 0:16],
                                                                      in1=U[p0:p0 + 64, c, 16:32], op=ALU.subtract),
                                     [Btmp, Bu[sl]], [Bpl])

                        S.op("dve", lambda e: e.tensor_tensor(out=TA[:, 1:W], in0=U[:, c, 1:W], in1=U[:, c, 0:W - 1], op=ALU.add), [Bu[sl]], [Bpt[0]])
                        S.op("dve", lambda e: e.tensor_tensor(out=TB[:, 3:W], in0=TA[:, 3:W], in1=TA[:, 1:W - 2], op=ALU.add), [Bpt[0]], [Bpt[1]])
                        if c == 0:
                            pool_out(0, TA, Bpt[0], 0.5)
                            pool_out(64, TB, Bpt[1], 0.25)
                        else:
                            S.op("dve", lambda e: e.tensor_tensor(out=TA[:, 7:W], in0=TB[:, 7:W], in1=TB[:, 3:W - 4], op=ALU.add), [Bpt[1]], [Bpt[0]])
                            pool_out(0, TA, Bpt[0], 0.125)
                            S.op("dve", lambda e: e.tensor_tensor(out=TB[:, 15:W], in0=TA[:, 15:W], in1=TA[:, 7:W - 8], op=ALU.add), [Bpt[0]], [Bpt[1]])
                            pool_out(64, TB, Bpt[1], 0.0625)
                    Yp = []
                    for c in range(2):
                        pb, Bpb = pjbank()
                        S.op("pe", lambda e: e.matmul(pb[:, :], lhsT=pw_bf[:, c, :], rhs=pooled[c][0][:, :], start=True, stop=True),
                             [Bpw, pooled[c][1]], [Bpb])
                        y, By = ytile[c], Bytile[c]
                        S.op("act", lambda e: e.activation(out=y[:], in_=pb[:, :], func=AF.Identity, scale=pcol(P_PSCALE + c)),
                             [Bpb, Bprm], [By])
                        Yp.append((y, By))

                    def rms_group(Ys, nfeat, ci0):
                        srcs = []
                        for (y, By) in Ys:
                            sq, Bsq = fp.get()
                            S.op("pool", lambda e: e.tensor_tensor(out=sq[:], in0=y[:], in1=y[:], op=ALU.mult), [By], [Bsq])
                            srcs.append((sq[:], Bsq))
                        r, Br = stats_rinv(srcs, 1.0 / nfeat, RMS_EPS, fp, STb, BST)
                        for i, (y, By) in enumerate(Ys):
                            ci = ci0 + i
                            S.op("dve", lambda e: e.scalar_tensor_tensor(out=yn[:, ci, :], in0=y[:], scalar=pcol(P_GMIX + ci), in1=r[:],
                                                                         op0=ALU.mult, op1=ALU.mult), [By, Br, Bprm], [Byn[ci]])

                    rms_group(Yp, 256, 0)

                    chk("pool1")
                    Yc = []
                    Rbank = ps[4]
                    for hp in range(3):
                        jlist = [j_ for j_ in (3, 4, 2, 5, 1, 6, 0, 7) if (t > 0 or j_ >= 4)]
                        units = [(hh, j) for j in jlist for hh in range(2)]
                        cu = {}
                        firstC = [True, True]

                        def ca(u):
                            hh, j = units[u]
                            h = 2 * hp + hh
                            hb = hh * 64
                            jsl = sl if j >= 4 else 1 - sl
                            kcol = jsl * TT + (j % 4) * 128
                            qc_lo = max(0, 2 * j - 8)
                            qc_hi = min(7, 2 * j + 1)
                            qlo, qhi = qc_lo * 64, qc_hi * 64 + 64
                            N = qhi - qlo
                            Dd = qlo + 512 - 128 * j
                            masked = ((not full) and t == 1 and j < 4)
                            zb, Bzb = Z[u % 2], BZ[u % 2]
                            S.op("pe", lambda e: e.matmul(zb[:, 0:N], lhsT=kT_c[hb:hb + 64, hp, kcol:kcol + 128],
                                                          rhs=qT_c[hb:hb + 64, hp, qlo:qhi], start=True, stop=True),
                                 [BkTc[jsl][hp], BqTc[hp]], [Bzb])
                            if j <= 2:
                                ntab = 0
                            elif j == 3:
                                ntab = 128
                            else:
                                ntab = min(256, N)
                            P, BP = bp.get()
                            if ntab > 0:
                                tt_, Btt = fp.get()
                                g0 = P_GTAB + h * 256 + Dd
                                S.op("dve", lambda e: e.tensor_tensor(out=tt_[:, 0:ntab], in0=zb[:, 0:ntab], in1=prm[:, g0:g0 + ntab], op=ALU.add),
                                     [Bzb, Bprm], [Btt])
                                if masked:
                                    S.op("act", lambda e: e.activation(out=P[:, 0:ntab], in_=tt_[:, 0:ntab], func=AF.Exp, bias=pcol(P_CMASK + 1)),
                                         [Btt, Bprm], [BP])
                                else:
                                    S.op("act", lambda e: e.activation(out=P[:, 0:ntab], in_=tt_[:, 0:ntab], func=AF.Exp), [Btt], [BP])
                            if N > ntab:
                                bias_ap = small[:, h:h + 1] if masked else pcol(P_BIASC + h)
                                S.op("act", lambda e: e.activation(out=P[:, ntab:N], in_=zb[:, ntab:N], func=AF.Exp, bias=bias_ap),
                                     [Bzb, Bprm, Bsmall], [BP])
                            if j <= 3:
                                c0 = (2 * j + 1) * 64 - qlo
                                S.op("pool", lambda e: e.memset(P[0:64, c0:c0 + 64], 0.0), [], [BP])
                            else:
                                S.op("pool", lambda e: e.memset(P[64:128, 0:64], 0.0), [], [BP])
                            cu[u] = (hh, j, h, hb, jsl, qlo, qhi, N, P, BP)

                        def cb_(u):
                            hh, j, h, hb, jsl, qlo, qhi, N, P, BP = cu.pop(u)
                            S.op("pe", lambda e: e.matmul(Obank[hb:hb + 64, qlo:qhi], lhsT=v_c[:, jsl * 4 + j % 4, h * 64:(h + 1) * 64], rhs=P[:, 0:N],
                                                          start=firstC[hh], stop=(j == 7), skip_group_check=True),
                                 [Bvc[jsl][j % 4], BP], [BO[hh]])
                            S.op("pe", lambda e: e.matmul(Rbank[hb:hb + 64, qlo:qhi], lhsT=cstb[:, C_ONES:C_ONES + 64], rhs=P[:, 0:N],
                                                          start=firstC[hh], stop=(j == 7), skip_group_check=True),
                                 [Bcstb, BP], [BR2[hh]])
                            firstC[hh] = False

                        nu = len(units)
                        ca(0)
                        if nu > 1:
                            ca(1)
                        for u in range(nu):
                            cb_(u)
                            if u + 2 < nu:
                                ca(u + 2)
                                S.op("pe", lambda e: e.matmul(ps[0][:, 0:128], lhsT=cstb[:, C_ONES:C_ONES + 128], rhs=cstb[:, 0:128],
                                                              start=True, stop=True), [Bcstb], [Bps[0]], signal=False)
                        rec, Brec = fp.get()
                        S.op("dve", lambda e: e.reciprocal(out=rec[:], in_=Rbank[:, :]), BR2, [Brec])
                        y, By = ytile[hp], Bytile[hp]
                        S.op("dve", lambda e: e.tensor_tensor(out=y[:], in0=Obank[:, :], in1=rec[:], op=ALU.mult), BO + [Brec], [By])
                        Yc.append((y, By))
                    rms_group(Yc, 384, 2)

                    chk("chunk1")
                    Ys = []
                    nkt = 4 * t + 4
                    for hp in range(3):
                        tasks = [(hh, kt) for kt in range(nkt - 1, -1, -1) for hh in range(2)]
                        st = {}
                        firstA = [True, True]
                        firstO = [True, True]

                        def s1(i):
                            hh, kt = tasks[i]
                            hb = hh * 64
                            r = kt - 4 * t
                            c0 = 128 * r if r >= 0 else 0
                            N = TT - c0
                            masked = (not full) and kt < 4
                            zb, Bzb = Z[i % 2], BZ[i % 2]
                            S.op("pe", lambda e: e.matmul(zb[:, 0:N], lhsT=kT_s[hb:hb + 64, hp, kt * 128:(kt + 1) * 128],
                                                          rhs=qT_s[hb:hb + 64, hp, c0:TT], start=True, stop=True),
                                 [BkTs[kt // 4][hp], BqTs[hp]], [Bzb])
                            if r >= 0:
                                S.op("pe", lambda e: e.matmul(zb[:, 0:128], lhsT=cstb[:, C_IDENT:C_IDENT + 128], rhs=cstb[:, C_NEG:C_NEG + 128],
                                                              start=False, stop=True, skip_group_check=True), [Bcstb], [Bzb])
                            zs, Bzs = zp.get()
                            S.op("dve", lambda e: e.tensor_copy(out=zs[:, 0:N], in_=zb[:, 0:N]), [Bzb], [Bzs])
                            E, BE = fp.get()
                            S.op("act", lambda e: e.activation(out=E[:, 0:N], in_=zb[:, 0:N], func=AF.Exp), [Bzb], [BE])
                            SP, BSP = bp.get()
                            if masked:
                                S.op("act", lambda e: e.activation(out=SP[:, 0:N], in_=E[:, 0:N], func=AF.Ln, scale=pcol(P_CMASK), bias=1.0),
                                     [BE, Bprm], [BSP])
                            else:
                                S.op("act", lambda e: e.activation(out=SP[:, 0:N], in_=E[:, 0:N], func=AF.Ln, bias=1.0), [BE], [BSP])
                            st[i] = dict(hh=hh, kt=kt, hb=hb, r=r, c0=c0, N=N, masked=masked, SP=SP, BSP=BSP, zs=zs, Bzs=Bzs)

                        def s2(i):
                            d = st[i]
                            hh, kt, hb, c0, N = d["hh"], d["kt"], d["hb"], d["c0"], d["N"]
                            zs, Bzs = d["zs"], d["Bzs"]
                            Ab, BAb = Abank[hh], BA[hh]
                            S.op("pe", lambda e: e.matmul(Ab[:, c0:TT], lhsT=cstb[:, C_TRI:C_TRI + 128], rhs=d["SP"][:, 0:N],
                                                          start=firstA[hh], stop=True, skip_group_check=True),
                                 [Bcstb, d["BSP"]], BAb)
                            firstA[hh] = False
                            S.op("dve", lambda e: e.tensor_tensor(out=zs[:, 0:N], in0=Ab[:, c0:TT], in1=zs[:, 0:N], op=ALU.subtract),
                                 BAb + [Bzs], [Bzs])
                            Wt, BW = bp.get()
                            if d["masked"]:
                                S.op("act", lambda e: e.activation(out=Wt[:, 0:N], in_=zs[:, 0:N], func=AF.Exp, scale=-1.0, bias=pcol(P_CMASK + 1)),
                                     [Bzs, Bprm], [BW])
                            else:
                                S.op("act", lambda e: e.activation(out=Wt[:, 0:N], in_=zs[:, 0:N], func=AF.Exp, scale=-1.0), [Bzs], [BW])
                            d["W"], d["BW"] = Wt, BW

                        def filler(nf_):
                            for _ in range(nf_):
                                S.op("pe", lambda e: e.matmul(ps[0][:, 0:256], lhsT=cstb[:, C_ONES:C_ONES + 128], rhs=cstb[:, 0:256],
                                                              start=True, stop=True), [Bcstb], [Bps[0]], signal=False)

                        def s2b(i):
                            d = st[i]
                            hh, kt, c0, N = d["hh"], d["kt"], d["c0"], d["N"]
                            Ab, BAb = Abank[hh], BA[hh]
                            filler(NFILL)
                            if kt > 0:
                                S.op("pe", lambda e: e.matmul(Ab[:, c0:TT], lhsT=cstb[:, C_TRIU:C_TRIU + 128], rhs=d["SP"][:, 0:N],
                                                              start=False, stop=True, skip_group_check=True),
                                     [Bcstb, d["BSP"]], BAb)

                        def s3(i):
                            d = st.pop(i)
                            hh, kt, hb, c0, N = d["hh"], d["kt"], d["hb"], d["c0"], d["N"]
                            h = 2 * hp + hh
                            last = (kt == 0)
                            S.op("pe", lambda e: e.matmul(Obank[hb:hb + 64, c0:TT], lhsT=v_s[:, kt, h * 64:(h + 1) * 64], rhs=d["W"][:, 0:N],
                                                          start=firstO[hh], stop=last, skip_group_check=True),
                                 [Bvs[kt // 4][kt % 4], d["BW"]], [BO[hh]])
                            firstO[hh] = False
                            filler(NFILL)

                        n = len(tasks)
                        for i in range(-2, n + 2):
                            if 0 <= i < n:
                                s2(i)
                            if 0 <= i + 2 < n:
                                s1(i + 2)
                            if 0 <= i - 1 < n:
                                s2b(i - 1)
                            if 0 <= i - 2 < n:
                                s3(i - 2)
                        y, By = ytile[hp], Bytile[hp]
                        S.op("dve", lambda e: e.tensor_copy(out=y[:], in_=Obank[:, :]), BO, [By])
                        Ys.append((y, By))
                    rms_group(Ys, 384, 5)

                    if debug:
                        Bd = Buf()
                        S.dma("sp", dbg_yn[:, :, own_i * TT:(own_i + 1) * TT], yn[:], Bd, reads=Byn, writes=[Bd])

                    chk("sb1")
                    for dch in range(8):
                        pb, Bpb = pjbank()
                        for k in range(8):
                            S.op("pe", lambda e: e.matmul(pb[:, :], lhsT=w_out_bf[:, k, dch * 128:(dch + 1) * 128], rhs=yn[:, k, :],
                                                          start=(k == 0), stop=(k == 7)),
                                 [Bwout, Byn[k]], [Bpb], signal=(k == 7))
                        S.op("dve", lambda e: e.scalar_tensor_tensor(out=xres[:, dch, :], in0=xres[:, dch, :], scalar=ALPHA, in1=pb[:, :],
                                                                     op0=ALU.mult, op1=ALU.add), [Bxres[dch], Bxres_d, Bpb], [Bxres[dch]])
                    layer_norm(lambda d_: xres[:, d_, :], Bxres, P_LN1G, P_LN1B, fp, STb, BST, ps[2], Bps[2], lnMR, BlnMR)
                    S.dma("sp", x1s_v[:, :, own_i * TT:(own_i + 1) * TT], xres[:], Bxres_d, reads=Bxres + [Bxres_d], writes=[Bx1s[own_i]])
                    own_i += 1
                chk("mixer")
                S.barrier()
                for e_ in ENGS:
                    for b in Bx1s:
                        if not S.dead:
                            S._wait(e_, b.w)

            for hf in range(2 if full else 1):
                pfx2 = "L%dh%d_" % (li, hf)
                cb = hf * 4 * TT
                with ExitStack() as es:
                    sb = lambda name, shape, dtp: es.enter_context(nc.sbuf_tensor(pfx2 + name, shape, dtp))
                    x1bf = sb("x1bf", [128, 8, 4 * TT], BF16)
                    acc = sb("acc", [128, 8, 4 * TT], F32)
                    NWS = 2
                    wg_bf = [sb("wg_bf%d" % i, [128, 8, 512], BF16) for i in range(NWS)]
                    wu_bf = [sb("wu_bf%d" % i, [128, 8, 512], BF16) for i in range(NWS)]
                    wd_bf = [sb("wd_bf%d" % i, [128, 4, D], BF16) for i in range(NWS)]
                    hact = [sb("hact%d" % i, [128, 4, TT], BF16) for i in range(2)]
                    fp = TPool(nc, es, pfx2 + "ffp", [128, 512], F32, 6)
                    lnMR = [sb("flnMR%d" % i, [128, TT], F32) for i in range(2)]
                    BlnMR = [Buf(), Buf()]
                    Bx1bf = [Buf() for i in range(4)]
                    Bacc = [[Buf() for i in range(4)] for d_ in range(8)]
                    Bwg = [Buf() for i in range(NWS)]
                    Bwu = [Buf() for i in range(NWS)]
                    Bwd = [Buf() for i in range(NWS)]
                    Bhact = [[Buf() for c in range(4)] for i in range(2)]
                    for i in range(4):
                        S.dma("pool", x1bf[:, :, i * TT:(i + 1) * TT], x1s_v[:, :, cb + i * TT:cb + (i + 1) * TT], Bx1bf[i], reads=[Bx1s[4 * hf + i]], writes=[Bx1bf[i]])

                    if moe:
                        gB = [sb("gB%d" % i, [128, 4 * TT], F32) for i in range(2)]
                        BgB = [[Buf() for i in range(4)] for _ in range(2)]
                        gates = sb("gates", [128, 16, NE], F32)
                        Bgates = Buf("gates")
                        rt = sb("rt", [128, 8, NE], F32)
                        Brt = Buf("rt")
                        x1f = [sb("x1f0", [128, 8, 128], F32)] * 2
                        Bx1f = [Buf()] * 2
                        gsm = sb("gsm", [128, 64], F32)
                        Bgsm = Buf()
                        gexp = [sb("gexp%d" % i, [128, 128], F32) for i in range(2)]
                        Bgexp = [Buf(), Buf()]
                        S.dma("sp", rt[:], router.rearrange("(k p) e -> p k e", p=128), Brt, writes=[Brt])
                        RB, BRB = ps[7], Bps[7]
                        for s in range(16):
                            xf, Bxf = x1f[s % 2], Bx1f[s % 2]
                            S.dma("sp", xf[:], x1s_v[:, :, cb + s * 128:cb + (s + 1) * 128], Bxf, reads=[Bx1s[4 * hf + s // 4]], writes=[Bxf])
                            for k in range(8):
                                S.op("pe", lambda e: e.matmul(RB[:, 0:NE], lhsT=xf[:, k, :], rhs=rt[:, k, :], start=(k == 0), stop=(k == 7)),
                                     [Bxf, Brt], [BRB])
                            lg = gsm[:, 0:8]
                            S.op("act", lambda e: e.activation(out=lg, in_=RB[:, 0:NE], func=AF.Copy), [BRB], [Bgsm])
                            m8 = gsm[:, 8:16]
                            S.op("dve", lambda e: e.max(out=m8, in_=lg), [Bgsm], [Bgsm])
                            dd = gsm[:, 16:17]
                            S.op("dve", lambda e: e.tensor_tensor(out=dd, in0=gsm[:, 9:10], in1=gsm[:, 8:9], op=ALU.subtract), [Bgsm], [Bgsm])
                            ee = gsm[:, 17:18]
                            S.op("act", lambda e: e.activation(out=ee, in_=dd, func=AF.Exp), [Bgsm], [Bgsm])
                            den = gsm[:, 18:19]
                            S.op("dve", lambda e: e.tensor_scalar(out=den, in0=ee, scalar1=1.0, scalar2=None, op0=ALU.add), [Bgsm], [Bgsm])
                            w1 = gsm[:, 19:20]
                            S.op("dve", lambda e: e.reciprocal(out=w1, in_=den), [Bgsm], [Bgsm])
                            w2 = gsm[:, 20:21]
                            S.op("dve", lambda e: e.tensor_tensor(out=w2, in0=ee, in1=w1, op=ALU.mult), [Bgsm], [Bgsm])
                            m1 = gsm[:, 24:32]
                            S.op("dve", lambda e: e.tensor_scalar(out=m1, in0=lg, scalar1=gsm[:, 8:9], scalar2=w1, op0=ALU.is_equal, op1=ALU.mult),
                                 [Bgsm], [Bgsm])
                            m2 = gsm[:, 32:40]
                            S.op("dve", lambda e: e.tensor_scalar(out=m2, in0=lg, scalar1=gsm[:, 9:10], scalar2=w2, op0=ALU.is_equal, op1=ALU.mult),
                                 [Bgsm], [Bgsm])
                            S.op("dve", lambda e: e.tensor_tensor(out=gates[:, s, :], in0=m1, in1=m2, op=ALU.add), [Bgsm], [Bgates])

                    GU = [(ps[0], Bps[0], ps[1], Bps[1]), (ps[2], Bps[2], ps[3], Bps[3])]
                    OBk = [(ps[4], Bps[4]), (ps[5], Bps[5]), (ps[6], Bps[6])]
                    groups = [(0, 4), (4, 4), (8, 4), (12, 4), (16, 4), (20, 2)]
                    ws_i = 0
                    gu_i = 0
                    ob_i = 0
                    ha_i = 0
                    first_acc = True
                    for ex in range(NE if moe else 1):
                        if moe:
                            gb, Bgb = gB[ex % 2], BgB[ex % 2]
                            for s in range(16):
                                ge, Bge = gexp[s % 2], Bgexp[s % 2]
                                S.op("dve", lambda e: e.tensor_scalar(out=ge[:], in0=onesf, scalar1=gates[:, s, ex:ex + 1], scalar2=None, op0=ALU.mult),
                                     [Bcstf, Bgates], [Bge])
                                S.op("pe", lambda e: e.matmul(ps[7][:, (s % 4) * 128:(s % 4 + 1) * 128], lhsT=ge[:], rhs=identf, start=True, stop=True),
                                     [Bge, Bcstf], [Bps[7]])
                                if s % 4 == 3:
                                    i4 = s // 4
                                    S.op("act", lambda e: e.activation(out=gb[:, i4 * TT:(i4 + 1) * TT], in_=ps[7][:, :], func=AF.Copy), [Bps[7]], [Bgb[i4]])
                        for (f0, nf) in groups:
                            w_i = ws_i % NWS
                            ws_i += 1
                            S.dma("pool", wg_bf[w_i][:, :, 0:nf * 128], wg[ex, 131072 * f0:131072 * (f0 + nf)].rearrange("(p k f) -> p k f", p=128, k=8),
                                  Bwg[w_i], writes=[Bwg[w_i]])
                            S.dma("pool", wu_bf[w_i][:, :, 0:nf * 128], wu[ex, 131072 * f0:131072 * (f0 + nf)].rearrange("(p k f) -> p k f", p=128, k=8),
                                  Bwu[w_i], writes=[Bwu[w_i]])
                            S.dma("pool", wd_bf[w_i][:, 0:nf, :], wd[ex, 131072 * f0:131072 * (f0 + nf)].rearrange("(p c d) -> p c d", p=128, c=nf),
                                  Bwd[w_i], writes=[Bwd[w_i]])
                            for i in range(4):
                                ha, Bha = hact[ha_i % 2], Bhact[ha_i % 2]
                                ha_i += 1
                                for fc in range(nf):
                                    G, BG, Ub, BU = GU[gu_i % 2]
                                    gu_i += 1
                                    for k in range(8):
                                        S.op("pe", lambda e: e.matmul(G[:, :], lhsT=wg_bf[w_i][:, k, fc * 128:(fc + 1) * 128], rhs=x1bf[:, k, i * TT:(i + 1) * TT],
                                                                      start=(k == 0), stop=(k == 7)), [Bwg[w_i], Bx1bf[i]], [BG], signal=(k == 7))
                                    for k in range(8):
                                        S.op("pe", lambda e: e.matmul(Ub[:, :], lhsT=wu_bf[w_i][:, k, fc * 128:(fc + 1) * 128], rhs=x1bf[:, k, i * TT:(i + 1) * TT],
                                                                      start=(k == 0), stop=(k == 7)), [Bwu[w_i], Bx1bf[i]], [BU], signal=(k == 7))
                                    sg, Bsg = fp.get()
                                    S.op("act", lambda e: e.activation(out=sg[:], in_=G[:, :], func=AF.Silu), [BG], [Bsg])
                                    if moe:
                                        S.op("dve", lambda e: e.tensor_tensor(out=sg[:], in0=sg[:], in1=Ub[:, :], op=ALU.mult), [Bsg, BU], [Bsg])
                                        S.op("dve", lambda e: e.tensor_tensor(out=ha[:, fc, :], in0=sg[:], in1=gb[:, i * TT:(i + 1) * TT], op=ALU.mult),
                                             [Bsg, Bgb[i]], [Bha[fc]])
                                    else:
                                        S.op("dve", lambda e: e.tensor_tensor(out=ha[:, fc, :], in0=sg[:], in1=Ub[:, :], op=ALU.mult), [Bsg, BU], [Bha[fc]])
                                for dch in range(8):
                                    ob, Bob = OBk[ob_i % 3]
                                    ob_i += 1
                                    for fc in range(nf):
                                        S.op("pe", lambda e: e.matmul(ob[:, :], lhsT=wd_bf[w_i][:, fc, dch * 128:(dch + 1) * 128], rhs=ha[:, fc, :],
                                                                      start=(fc == 0), stop=(fc == nf - 1)), [Bwd[w_i], Bha[fc]], [Bob], signal=(fc == nf - 1))
                                    if first_acc:
                                        S.op("act", lambda e: e.activation(out=acc[:, dch, i * TT:(i + 1) * TT], in_=ob[:, :], func=AF.Copy), [Bob], [Bacc[dch][i]])
                                    else:
                                        S.op("dve", lambda e: e.tensor_tensor(out=acc[:, dch, i * TT:(i + 1) * TT], in0=acc[:, dch, i * TT:(i + 1) * TT],
                                                                              in1=ob[:, :], op=ALU.add), [Bacc[dch][i], Bob], [Bacc[dch][i]])
                            first_acc = False

                    for i in range(4):
                        for dch in range(8):
                            xt_, Bxt = fp.get()
                            S.dma("sp", xt_[:], x1s[dch * 128:(dch + 1) * 128, cb + i * TT:cb + (i + 1) * TT], Bxt, reads=[Bx1s[4 * hf + i]], writes=[Bxt])
                            S.op("dve", lambda e: e.scalar_tensor_tensor(out=acc[:, dch, i * TT:(i + 1) * TT], in0=xt_[:], scalar=ALPHA,
                                                                         in1=acc[:, dch, i * TT:(i + 1) * TT], op0=ALU.mult, op1=ALU.add),
                                 [Bxt, Bacc[dch][i]], [Bacc[dch][i]])
                        Bv = [Bacc[d_][i] for d_ in range(8)]
                        layer_norm(lambda d_: acc[:, d_, i * TT:(i + 1) * TT], Bv, P_LN2G, P_LN2B, fp, ps[7], Bps[7], ps[6], Bps[6], lnMR, BlnMR)
                        Bo_sb = Buf()
                        S.dma("sp", dst_v[:, :, cb + i * TT:cb + (i + 1) * TT], acc[:, :, i * TT:(i + 1) * TT], Bo_sb, reads=Bv, writes=[Bdst[4 * hf + i]])
                S.barrier()
                for e_ in ENGS:
                    for b in Bdst[4 * hf:4 * hf + 4]:
                        if not S.dead:
                            S._wait(e_, b.w)

        Bxin = [Buf() for t in range(NT)]
        Bx2s = [Buf("x2s%d" % t) for t in range(NT)]
        Bxs2 = [Buf("xs2%d" % t) for t in range(NT)]
        emit_layer(0, False, True, xT_v, Bxin, x2s_v, Bx2s)
        with ExitStack() as es:
            S.dma("sp", prm[:], prm_all[1], Bprm, writes=[Bprm])
            bl = TPool(nc, es, "bl", [128, 8, TT], F32, 4)
            for t in range(NT):
                a1, Ba1 = bl.get()
                S.dma("sp", a1[:], x2s_v[:, :, t * TT:(t + 1) * TT], Ba1, reads=[Bx2s[t]], writes=[Ba1])
                S.op("dve", lambda e: e.tensor_scalar(out=a1[:], in0=a1[:], scalar1=pcol(P_CMASK), scalar2=None, op0=ALU.mult), [Ba1, Bprm], [Ba1])
                if t > 0:
                    a0, Ba0 = bl.get()
                    S.dma("sp", a0[:], x2s_v[:, :, (t - 1) * TT:t * TT], Ba0, reads=[Bx2s[t - 1]], writes=[Ba0])
                    S.op("dve", lambda e: e.scalar_tensor_tensor(out=a1[:], in0=a0[:], scalar=pcol(P_M0), in1=a1[:], op0=ALU.mult, op1=ALU.add),
                         [Ba0, Ba1, Bprm], [Ba1])
                S.dma("sp", xs2_v[:, :, t * TT:(t + 1) * TT], a1[:], Ba1, reads=[Ba1], writes=[Bxs2[t]])
            S.barrier()
            for e_ in ENGS:
                for b in Bxs2:
                    S._wait(e_, b.w)
        emit_layer(1, True, False, xs2_v, Bxs2, outT_v, Bout + [Buf() for _ in range(4)])
        S.finish(Bout)
        print("built fused: ops=%d waits=%d dma_sems=%d" % (S.nop, S.nwait, S.ndsem))
    return nc


def _consts():
    c = np.zeros((128, NCST), np.float32)
    j = np.arange(128)[:, None]
    k = np.arange(128)[None, :]
    c[:, C_TRI:C_TRI + 128] = (j >= k)
    c[:, C_TRIU:C_TRIU + 128] = (j < k)
    c[:, C_ONES:C_ONES + 128] = 1.0
    c[:, C_IDENT:C_IDENT + 128] = np.eye(128)
    c[:, C_NEG:C_NEG + 128] = np.where(j >= k, -100.0, 0.0)
    return c


def _cols(v):
    return np.ascontiguousarray(v.reshape(8, 128).T)


def _prm(j, li, full, pool_scale, g_mix, ln1_g, ln1_b, ln2_g, ln2_b, rel_bias):
    p = np.zeros((128, NPRM), np.float32)
    p[:, P_PSCALE:P_PSCALE + 2] = pool_scale[li].reshape(2, 128).T
    p[:, P_GMIX:P_GMIX + 8] = _cols(g_mix[li])
    p[:, P_LN1G:P_LN1G + 8] = _cols(ln1_g[li])
    p[:, P_LN1B:P_LN1B + 8] = _cols(ln1_b[li])
    p[:, P_LN2G:P_LN2G + 8] = _cols(ln2_g[li])
    p[:, P_LN2B:P_LN2B + 8] = _cols(ln2_b[li])
    rb = rel_bias[li]
    p[:, P_BIASC:P_BIASC + 6] = rb[:, 256][None, :]
    real = full or j == 1
    p[:, P_CMASK] = 1.0 if real else 0.0
    p[:, P_CMASK + 1] = 0.0 if real else -100.0
    p[:, P_M0] = 0.0 if j == 1 else 1.0
    wins = np.array([2, 4, 8, 16], np.float32)
    tau = np.arange(16, dtype=np.float32)
    seq_start = full or j == 0
    for c in range(2):
        for half in range(2):
            w = wins[c * 2 + half]
            if seq_start:
                ic = 1.0 / np.minimum(tau + 1.0, w)
            else:
                ic = np.full(16, 1.0 / w, np.float32)
            p[half * 64:(half + 1) * 64, P_ICNT + c * 16:P_ICNT + c * 16 + 16] = ic[None, :]
    pp = np.arange(128)[:, None]
    uu = np.arange(256)[None, :]
    idx = np.clip(uu - pp, -128, 128) + 128
    for h in range(6):
        p[:, P_GTAB + h * 256:P_GTAB + (h + 1) * 256] = rb[h][idx]
    return p


_GROUPS = [(0, 4), (4, 4), (8, 4), (12, 4), (16, 4), (20, 2)]


def _relayout_gu(w):
    E = w.shape[0]
    out = np.empty((E, D * DFF), np.float32)
    for e in range(E):
        w3 = w[e].reshape(8, 128, DFF)
        pos = 0
        for (f0, nf) in _GROUPS:
            blk = w3[:, :, f0 * 128:(f0 + nf) * 128].transpose(1, 0, 2)
            out[e, pos:pos + blk.size] = blk.reshape(-1)
            pos += blk.size
    return out


def _relayout_d(w):
    E = w.shape[0]
    out = np.empty((E, DFF * D), np.float32)
    for e in range(E):
        pos = 0
        for (f0, nf) in _GROUPS:
            blk = w[e][f0 * 128:(f0 + nf) * 128].reshape(nf, 128, D).transpose(1, 0, 2)
            out[e, pos:pos + blk.size] = blk.reshape(-1)
            pos += blk.size
    return out


_NC = []


def kernel(**inputs):
    inp = {k: np.asarray(v) for k, v in inputs.items()}
    x = np.ascontiguousarray(inp["x"], dtype=np.float32)
    if not _NC:
        _NC.append(build_fused())
    nc = _NC[0]
    cst = _consts()
    pwbd = np.zeros((2, 128, 2, 128), np.float32)
    for li in range(2):
        for c in range(2):
            pwbd[li, 0:64, c, 0:64] = inp["pool_w"][li][2 * c]
            pwbd[li, 64:128, c, 64:128] = inp["pool_w"][li][2 * c + 1]
    wg_r, wu_r, wd_r = _relayout_gu(inp["ffn_wg"]), _relayout_gu(inp["ffn_wu"]), _relayout_d(inp["ffn_wd"])
    mwg_r, mwu_r, mwd_r = _relayout_gu(inp["moe_wg"][0]), _relayout_gu(inp["moe_wu"][0]), _relayout_d(inp["moe_wd"][0])
    in_maps = []
    for core in range(8):
        b, j = core // 2, core % 2
        prm = np.stack([_prm(j, li, li == 0, inp["pool_scale"], inp["g_mix"], inp["ln1_g"], inp["ln1_b"], inp["ln2_g"],
                             inp["ln2_b"], inp["rel_bias"]) for li in range(2)])
        in_maps.append({"xT": np.ascontiguousarray(x[b].T), "w_in": inp["w_in"], "w_out": inp["w_out"], "pwbd": pwbd,
                        "prm": prm, "cst": cst, "wg": wg_r, "wu": wu_r, "wd": wd_r, "mwg": mwg_r, "mwu": mwu_r, "mwd": mwd_r,
                        "router": inp["moe_router"][0]})
    res = run_bass_kernel_spmd(nc, in_maps, core_ids=list(range(8)))
    out = np.empty_like(x)
    for core in range(8):
        b, j = core // 2, core % 2
        o = res.results[core]["outT"]
        for i in range(4):
            g = 2 * i + j
            out[b, g * TT:(g + 1) * TT, :] = o[:, i * TT:(i + 1) * TT].T
    return out
```

```python
import numpy as np
from contextlib import ExitStack
import concourse.bass as bass
import concourse.mybir as mybir
from concourse.bass_utils import run_bass_kernel_spmd

F32 = mybir.dt.float32
BF16 = mybir.dt.bfloat16
AF = mybir.ActivationFunctionType
ALU = mybir.AluOpType

ENGS = ("pe", "act", "dve", "pool", "sp")

D = 1024
SEQ = 4096
TT = 512
NT = 8
DFF = 2816
NE = 8
ALPHA = 4.0 ** 0.25
LN_EPS = 1e-5
RMS_EPS = 1e-6
OWN = (1, 3, 5, 7)
NFILL = 2

P_PSCALE = 0
P_GMIX = 2
P_LN1G = 10
P_LN1B = 18
P_LN2G = 26
P_LN2B = 34
P_BIASC = 42
P_CMASK = 48
P_ICNT = 50
P_GTAB = 82
P_M0 = 82 + 6 * 256
NPRM = P_M0 + 1
C_TRI = 0
C_TRIU = 128
C_ONES = 256
C_MASK = 384
C_IDENT = 512
NCST = 640


class Buf:
    __slots__ = ("name", "w", "rd", "dsem", "dcnt", "dkey", "excl")

    def __init__(self, name="", excl=False):
        self.name = name
        self.excl = excl
        self.w = None
        self.rd = []
        self.dsem = None
        self.dcnt = 0
        self.dkey = None


class Sched:
    def __init__(self, nc, es):
        self.nc = nc
        self.es = es
        self.eng = dict(pe=nc.tensor, act=nc.scalar, dve=nc.vector, pool=nc.gpsimd, sp=nc.sync)
        self.semh = {}
        for e in ENGS:
            self.semh[e] = es.enter_context(nc.semaphore("s_" + e))
        self.cnt = {e: 0 for e in ENGS}
        self.pending = {e: False for e in ENGS}
        self.seen = {e: {} for e in ENGS}
        self.snap = {}
        self.nwait = 0
        self.nop = 0
        self.ndsem = 0
        self.dead = False
        self.dbufs = []

    def _wait(self, e, t):
        if t is None:
            return
        key, val = t
        if key == e and e == "pe":
            return
        sn = self.seen[e]
        if sn.get(key, 0) >= val:
            return
        if key == e and self.cnt[e] < val:
            raise RuntimeError("self-wait on pending ticket " + e)
        self.eng[e].wait_ge(self.semh[key], val)
        self.nwait += 1
        sn[key] = val
        s = self.snap.get(t)
        if s is not None:
            for k, v in s:
                if sn.get(k, 0) < v:
                    sn[k] = v

    def _deps(self, e, reads, writes):
        for b in reads:
            self._wait(e, b.w)
        for b in writes:
            self._wait(e, b.w)
            for t in b.rd:
                self._wait(e, t)

    def _commit(self, t, reads, writes):
        for b in reads:
            b.rd.append(t)
            if len(b.rd) > 64:
                last = {}
                for k, v in b.rd:
                    if last.get(k, 0) < v:
                        last[k] = v
                b.rd = list(last.items())
        for b in writes:
            b.w = t
            b.rd = []

    def op(self, e, fn, reads=(), writes=(), signal=True):
        if self.dead:
            return None
        ex = [b for b in reads if b.excl]
        if ex:
            writes = list(writes) + ex
        self._deps(e, reads, writes)
        ins = fn(self.eng[e])
        self.nop += 1
        if signal:
            ins.then_inc(self.semh[e], 1)
            self.cnt[e] += 1
            t = (e, self.cnt[e])
            self.pending[e] = False
            sn = self.seen[e]
            self.snap[t] = tuple((k, sn.get(k, 0)) for k in ENGS if k != e and sn.get(k, 0) > 0)
        else:
            t = (e, self.cnt[e] + 1)
            self.pending[e] = True
        self._commit(t, reads, writes)
        return t

    def dma(self, q, out, in_, sb, reads=(), writes=(), **kw):
        if self.dead:
            return None
        self._deps(q, reads, writes)
        if sb.dsem is None:
            sb.dsem = self.es.enter_context(self.nc.semaphore("d%d" % self.ndsem))
            sb.dkey = ("d", self.ndsem)
            self.semh[sb.dkey] = sb.dsem
            self.ndsem += 1
            self.dbufs.append(sb)
        ins = self.eng[q].dma_start(out=out, in_=in_, **kw)
        ins.then_inc(sb.dsem, 16)
        sb.dcnt += 16
        t = (sb.dkey, sb.dcnt)
        self._commit(t, reads, writes)
        return t

    def barrier(self):
        if self.dead:
            return
        ts = [(e, self.cnt[e]) for e in ENGS if self.cnt[e] > 0]
        for e in ENGS:
            for t in ts:
                if t[0] != e:
                    self._wait(e, t)

    def finish(self, bufs):
        for b in self.dbufs:
            self._wait("sp", (b.dkey, b.dcnt))
        for b in bufs:
            self._wait("sp", b.w)
            for t in b.rd:
                self._wait("sp", t)
        for e in ENGS:
            assert not self.pending[e], e


class TPool:
    def __init__(self, nc, es, name, shape, dtype, n):
        self.t = [es.enter_context(nc.sbuf_tensor("%s%d" % (name, i), shape, dtype)) for i in range(n)]
        self.b = [Buf("%s%d" % (name, i)) for i in range(n)]
        self.i = 0

    def get(self):
        i = self.i
        self.i = (i + 1) % len(self.t)
        return self.t[i], self.b[i]


class _Stop(Exception):
    pass


def build_fused(debug=False, stage=None):
    nc = bass.Bass("TRN2", target_bir_lowering=False)

    def chk(name):
        if stage == name:
            S.dead = True
    dt = nc.dram_tensor
    xT = dt("xT", [D, SEQ], F32, kind="ExternalInput").ap()
    w_in_all = dt("w_in", [2, D, 2560], F32, kind="ExternalInput").ap()
    w_out_all = dt("w_out", [2, D, D], F32, kind="ExternalInput").ap()
    pwbd_all = dt("pwbd", [2, 128, 2, 128], F32, kind="ExternalInput").ap()
    prm_all = dt("prm", [2, 128, NPRM], F32, kind="ExternalInput").ap()
    cst_d = dt("cst", [128, NCST], F32, kind="ExternalInput").ap()
    wg_d = dt("wg", [1, D * DFF], F32, kind="ExternalInput").ap()
    wu_d = dt("wu", [1, D * DFF], F32, kind="ExternalInput").ap()
    wd_d = dt("wd", [1, DFF * D], F32, kind="ExternalInput").ap()
    wg_m = dt("mwg", [NE, D * DFF], F32, kind="ExternalInput").ap()
    wu_m = dt("mwu", [NE, D * DFF], F32, kind="ExternalInput").ap()
    wd_m = dt("mwd", [NE, DFF * D], F32, kind="ExternalInput").ap()
    router = dt("router", [D, NE], F32, kind="ExternalInput").ap()
    outT = dt("outT", [D, 4 * TT], F32, kind="ExternalOutput").ap()
    x1s = dt("x1s", [D, SEQ], F32, kind="Internal").ap()
    x2s = dt("x2s", [D, SEQ], F32, kind="Internal").ap()
    xs2 = dt("xs2", [D, SEQ], F32, kind="Internal").ap()

    xT_v = xT.rearrange("(k p) n -> p k n", p=128)
    outT_v = outT.rearrange("(k p) n -> p k n", p=128)
    x1s_v = x1s.rearrange("(k p) n -> p k n", p=128)
    x2s_v = x2s.rearrange("(k p) n -> p k n", p=128)
    xs2_v = xs2.rearrange("(k p) n -> p k n", p=128)

    with ExitStack() as es0:
        S = Sched(nc, es0)
        ps = [es0.enter_context(nc.psum_tensor("ps%d" % i, [128, 512], F32)) for i in range(8)]
        Bps = [Buf("ps%d" % i, True) for i in range(8)]
        prm = es0.enter_context(nc.sbuf_tensor("prm_sb", [128, NPRM], F32))
        cstf = es0.enter_context(nc.sbuf_tensor("cstf", [128, 256], F32))
        cstb = es0.enter_context(nc.sbuf_tensor("cstb", [128, NCST], BF16))
        Bprm, Bcstf, Bcstb = Buf("prm"), Buf("cstf"), Buf("cstb")
        S.dma("sp", cstf[:, 0:128], cst_d[:, C_ONES:C_ONES + 128], Bcstf, writes=[Bcstf])
        S.dma("sp", cstf[:, 128:256], cst_d[:, C_IDENT:C_IDENT + 128], Bcstf, writes=[Bcstf])
        S.dma("pool", cstb[:], cst_d, Bcstb, writes=[Bcstb])
        Bout = [Buf("out%d" % i) for i in range(4)]
        onesf = cstf[:, 0:128]
        identf = cstf[:, 128:256]

        def pcol(c, n=1):
            return prm[:, c:c + n]

        def stats_rinv(srcs, scale, eps, fp, bank, Bbank):
            n = len(srcs)
            for i, (a, b) in enumerate(srcs):
                S.op("pe", lambda e: e.matmul(bank[:, :], lhsT=onesf, rhs=a, start=(i == 0), stop=(i == n - 1)),
                     [Bcstf, b], [Bbank])
            l, Bl = fp.get()
            S.op("act", lambda e: e.activation(out=l[:], in_=bank[:, :], func=AF.Ln, scale=scale, bias=eps), [Bbank], [Bl])
            r, Br = fp.get()
            S.op("act", lambda e: e.activation(out=r[:], in_=l[:], func=AF.Exp, scale=-0.5), [Bl], [Br])
            return r, Br

        def layer_norm(vch, Bv, gcol, bcol, fp, bankA, BbankA, bankB, BbankB, MR, BMR):
            sqs = []
            for d in range(8):
                S.op("pe", lambda e: e.matmul(bankA[:, :], lhsT=onesf, rhs=vch(d), start=(d == 0), stop=(d == 7)),
                     [Bcstf, Bv[d]], [BbankA])
            for d in range(8):
                sq, Bsq = fp.get()
                S.op("pool", lambda e: e.tensor_tensor(out=sq[:], in0=vch(d), in1=vch(d), op=ALU.mult), [Bv[d]], [Bsq])
                S.op("pe", lambda e: e.matmul(bankB[:, :], lhsT=onesf, rhs=sq[:], start=(d == 0), stop=(d == 7)),
                     [Bcstf, Bsq], [BbankB])
            M, BM = MR[0], BMR[0]
            S.op("act", lambda e: e.activation(out=M[:], in_=bankA[:, :], func=AF.Identity, scale=1.0 / D), [BbankA], [BM])
            msq, Bmsq = fp.get()
            S.op("dve", lambda e: e.tensor_tensor(out=msq[:], in0=M[:], in1=M[:], op=ALU.mult), [BM], [Bmsq])
            V, BV = fp.get()
            S.op("dve", lambda e: e.scalar_tensor_tensor(out=V[:], in0=bankB[:, :], scalar=1.0 / D, in1=msq[:],
                                                         op0=ALU.mult, op1=ALU.subtract), [BbankB, Bmsq], [BV])
            L, BL = fp.get()
            S.op("act", lambda e: e.activation(out=L[:], in_=V[:], func=AF.Ln, bias=LN_EPS), [BV], [BL])
            Rr, BR = MR[1], BMR[1]
            S.op("act", lambda e: e.activation(out=Rr[:], in_=L[:], func=AF.Exp, scale=-0.5), [BL], [BR])
            for d in range(8):
                t1, Bt1 = fp.get()
                S.op("pool", lambda e: e.tensor_tensor(out=t1[:], in0=vch(d), in1=M[:], op=ALU.subtract), [Bv[d], BM], [Bt1])
                S.op("dve", lambda e: e.tensor_tensor(out=t1[:], in0=t1[:], in1=Rr[:], op=ALU.mult), [Bt1, BR], [Bt1])
                S.op("act", lambda e: e.activation(out=vch(d), in_=t1[:], func=AF.Identity,
                                                   scale=pcol(gcol + d), bias=pcol(bcol + d)), [Bt1, Bprm], [Bv[d]])

        def emit_layer(li, moe, full, xsrc_v, Bxsrc, dst_v, Bdst):
            pfx = "L%d_" % li
            w_in_v = w_in_all[li].rearrange("(k p) f -> p k f", p=128)
            w_out_v = w_out_all[li].rearrange("(k p) f -> p k f", p=128)
            pwbd = pwbd_all[li]
            wg, wu, wd = (wg_m, wu_m, wd_m) if moe else (wg_d, wu_d, wd_d)
            S.dma("sp", prm[:], prm_all[li], Bprm, writes=[Bprm])
            Bx1s = [Buf("x1s%d" % i) for i in range(8)]
            with ExitStack() as es:
                sb = lambda name, shape, dtp: es.enter_context(nc.sbuf_tensor(pfx + name, shape, dtp))
                w_in_bf = sb("w_in_bf", [128, 8, 2560], BF16)
                w_out_bf = sb("w_out_bf", [128, 8, D], BF16)
                pw_bf = sb("pw_bf", [128, 2, 128], BF16)
                kT_s = sb("kT_s", [128, 3, SEQ], BF16)
                v_s = sb("v_s", [128, 32, 384], BF16)
                kT_c = sb("kT_c", [128, 3, 2 * TT], BF16)
                v_c = sb("v_c", [128, 8, 384], BF16)
                qT_c = sb("qT_c", [128, 3, TT], BF16)
                qT_s = sb("qT_s", [128, 3, TT], BF16)
                xbf = [sb("xbf0", [128, 8, TT], BF16)] * 2
                ubuf = [sb("ubuf%d" % i, [128, 2, 16 + TT], F32) for i in range(2)]
                ytile = [sb("ytile%d" % i, [128, TT], F32) for i in range(3)]
                Bytile = [Buf() for i in range(3)]
                lnMR = [sb("lnMR%d" % i, [128, TT], F32) for i in range(2)]
                BlnMR = [Buf(), Buf()]
                yn = sb("yn", [128, 8, TT], BF16)
                xres = sb("xres", [128, 8, TT], F32)
                small = sb("small", [128, 32], F32)
                fp = TPool(nc, es, pfx + "fp", [128, 512], F32, 5)
                zp = TPool(nc, es, pfx + "zp", [128, 16 + TT], F32, 4)
                bp = TPool(nc, es, pfx + "bp", [128, 512], BF16, 8)

                Bwin = [Buf("win%d" % i) for i in range(4)]
                Bwout, Bpw = Buf("wout"), Buf("pw")
                BkTs = [[Buf() for c in range(3)] for t in range(NT)]
                Bvs = [[Buf() for s in range(4)] for t in range(NT)]
                BkTc = [[Buf() for c in range(3)] for sl in range(2)]
                Bvc = [[Buf() for s in range(4)] for sl in range(2)]
                BqTc = [Buf() for c in range(3)]
                BqTs = [Buf() for c in range(3)]
                BqTn = [Buf() for c in range(3)]
                Bxbf = [Buf("xbf0")] * 2
                Bu = [Buf("u0"), Buf("u1")]
                Bpt = [Buf("pt0"), Buf("pt1")]
                Byn = [Buf() for c in range(8)]
                Bxres = [Buf() for c in range(8)]
                Bxres_d = Buf("xres_d")
                Bsmall = Buf("small")

                for c in range(4):
                    S.dma("pool", w_in_bf[:, :, c * 640:(c + 1) * 640], w_in_v[:, :, c * 640:(c + 1) * 640], Bwin[c], writes=[Bwin[c]])
                S.dma("pool", pw_bf[:], pwbd, Bpw, writes=[Bpw])
                S.op("dve", lambda e: e.tensor_scalar(out=small[:, 0:6], in0=pcol(P_BIASC, 6), scalar1=pcol(P_CMASK + 1),
                                                      scalar2=None, op0=ALU.add), [Bprm], [Bsmall])
                S.op("dve", lambda e: e.memset(ubuf[0][:, :, 0:16], 0.0), [], [Bu[0]])

                pj_i = [0]

                def pjbank():
                    i = pj_i[0]
                    pj_i[0] = 1 - i
                    return ps[i], Bps[i]

                ev_i = [0]

                def evac(out, in_, reads, writes, scale=None):
                    ev_i[0] ^= 1
                    if ev_i[0]:
                        if scale is None:
                            S.op("act", lambda e: e.activation(out=out, in_=in_, func=AF.Copy), reads, writes)
                        else:
                            S.op("act", lambda e: e.activation(out=out, in_=in_, func=AF.Identity, scale=scale), reads, writes)
                    else:
                        if scale is None:
                            S.op("dve", lambda e: e.tensor_copy(out=out, in_=in_), reads, writes)
                        else:
                            S.op("dve", lambda e: e.tensor_scalar(out=out, in0=in_, scalar1=scale, scalar2=None, op0=ALU.mult),
                                 reads, writes)

                Z = [ps[2], ps[3]]
                BZ = [Bps[2], Bps[3]]
                Abank = [ps[4], ps[5]]
                B4 = [Buf("ps4lo", True), Buf("ps4hi", True)]
                BA = [B4, [Bps[5]]]
                Obank = ps[6]
                BO = [Buf("O_lo", True), Buf("O_hi", True)]
                STb, BST = ps[7], Bps[7]
                BR2 = B4

                own_i = 0
                chk("setup")
                for t in range(NT):
                    if t == 1:
                        chk("proj0")
                    if t == 2:
                        chk("own1")
                    own = full or (t in OWN)
                    first_own = (t == (0 if full else 1))
                    sl = t % 2
                    xb, Bxb = xbf[sl], Bxbf[sl]
                    if t == 0:
                        S.dma("pool", xb[:], xsrc_v[:, :, 0:TT], Bxb, reads=[Bxsrc[0]], writes=[Bxb])
                        S.dma("pool", w_out_bf[:], w_out_v, Bwout, writes=[Bwout])
                    if own:
                        S.dma("sp", xres[:], xsrc_v[:, :, t * TT:(t + 1) * TT], Bxres_d, reads=[Bxsrc[t]], writes=Bxres + [Bxres_d])

                    def proj_fm(col, out_fn):
                        pb, Bpb = pjbank()
                        for k in range(8):
                            S.op("pe", lambda e: e.matmul(pb[:, :], lhsT=w_in_bf[:, k, col:col + 128], rhs=xb[:, k, :],
                                                          start=(k == 0), stop=(k == 7)),
                                 [Bwin[col // 640], Bxb], [Bpb], signal=(k == 7))
                        out_fn(pb, Bpb)

                    if t == 1:
                        chk("t1a")
                    U = ubuf[sl]
                    for c in range(2):
                        proj_fm(c * 128, lambda pb, Bpb: evac(U[:, c, 16:16 + TT], pb[:, :], [Bpb], [Bu[sl]]))
                    if t < NT - 1:
                        S.op("dve", lambda e: e.tensor_copy(out=ubuf[1 - sl][:, :, 0:16], in_=U[:, :, TT:TT + 16]), [Bu[sl]], [Bu[1 - sl]])
                    for c in range(3):
                        proj_fm(640 + c * 128, lambda pb, Bpb: evac(kT_c[:, c, sl * TT:(sl + 1) * TT], pb[:, :], [Bpb], [BkTc[sl][c]]))
                    for c in range(3):
                        proj_fm(1792 + c * 128, lambda pb, Bpb: evac(kT_s[:, c, t * TT:(t + 1) * TT], pb[:, :], [Bpb], [BkTs[t][c]]))
                    if t == 1:
                        chk("t1b")
                    if own:
                        for c in range(3):
                            proj_fm(256 + c * 128, lambda pb, Bpb: evac(qT_c[:, c, :], pb[:, :], [Bpb], [BqTc[c]], scale=0.125))
                        for c in range(3):
                            proj_fm(1408 + c * 128, lambda pb, Bpb: evac(qT_s[:, c, :], pb[:, :], [Bpb], [BqTs[c]], scale=0.125))
                    if t == 1:
                        chk("t1c")
                    for s in range(4):
                        for (col, dst, Bd) in ((1024, v_c[:, sl * 4 + s, :], Bvc[sl][s]), (2176, v_s[:, t * 4 + s, :], Bvs[t][s])):
                            pb, Bpb = pjbank()
                            wb = sorted(set([col // 640, (col + 383) // 640]))
                            for k in range(8):
                                S.op("pe", lambda e: e.matmul(pb[:, 0:384], lhsT=xb[:, k, s * 128:(s + 1) * 128], rhs=w_in_bf[:, k, col:col + 384],
                                                              start=(k == 0), stop=(k == 7)),
                                     [Bwin[i] for i in wb] + [Bxb], [Bpb], signal=(k == 7))
                            evac(dst, pb[:, 0:384], [Bpb], [Bd])
                    if t + 1 < NT:
                        S.dma("pool", xb[:], xsrc_v[:, :, (t + 1) * TT:(t + 2) * TT], Bxb, reads=[Bxsrc[t + 1]], writes=[Bxb])
                    if not own:
                        continue

                    chk("proj1")
                    W = 16 + TT
                    (TA, BTA), (TB, BTB) = zp.get(), zp.get()
                    Bpt = [BTA, BTB]
                    pooled = []
                    for c in range(2):
                        pl, Bpl = bp.get()
                        pooled.append((pl, Bpl))

                        def pool_out(p0, Tsrc, BT, inv):
                            S.op("dve", lambda e: e.scalar_tensor_tensor(out=pl[p0:p0 + 64, :], in0=Tsrc[p0:p0 + 64, 16:W], scalar=inv,
                                                                         in1=U[p0:p0 + 64, c, 16:W], op0=ALU.mult, op1=ALU.subtract),
                                 [BT, Bu[sl]], [Bpl])
                            if first_own:
                                tmp, Btmp = fp.get()
                                S.op("dve", lambda e: e.tensor_tensor(out=tmp[p0:p0 + 64, 0:16], in0=Tsrc[p0:p0 + 64, 16:32],
                                                                      in1=prm[p0:p0 + 64, P_ICNT + c * 16:P_ICNT + c * 16 + 16], op=ALU.mult),
                                     [BT, Bprm], [Btmp])
                                S.op("dve", lambda e: e.tensor_tensor(out=pl[p0:p0 + 64, 0:16], in0=tmp[p0:p0 + 64, 0:16],
                                                                      in1=U[p0:p0 + 64, c, 16:32], op=ALU.subtract),
                                     [Btmp, Bu[sl]], [Bpl])

                        S.op("dve", lambda e: e.tensor_tensor(out=TA[:, 1:W], in0=U[:, c, 1:W], in1=U[:, c, 0:W - 1], op=ALU.add), [Bu[sl]], [Bpt[0]])
                        S.op("dve", lambda e: e.tensor_tensor(out=TB[:, 3:W], in0=TA[:, 3:W], in1=TA[:, 1:W - 2], op=ALU.add), [Bpt[0]], [Bpt[1]])
                        if c == 0:
                            pool_out(0, TA, Bpt[0], 0.5)
                            pool_out(64, TB, Bpt[1], 0.25)
                        else:
                            S.op("dve", lambda e: e.tensor_tensor(out=TA[:, 7:W], in0=TB[:, 7:W], in1=TB[:, 3:W - 4], op=ALU.add), [Bpt[1]], [Bpt[0]])
                            pool_out(0, TA, Bpt[0], 0.125)
                            S.op("dve", lambda e: e.tensor_tensor(out=TB[:, 15:W], in0=TA[:, 15:W], in1=TA[:, 7:W - 8], op=ALU.add), [Bpt[0]], [Bpt[1]])
                            pool_out(64, TB, Bpt[1], 0.0625)
                    Yp = []
                    for c in range(2):
                        pb, Bpb = pjbank()
                        S.op("pe", lambda e: e.matmul(pb[:, :], lhsT=pw_bf[:, c, :], rhs=pooled[c][0][:, :], start=True, stop=True),
                             [Bpw, pooled[c][1]], [Bpb])
                        y, By = ytile[c], Bytile[c]
                        S.op("act", lambda e: e.activation(out=y[:], in_=pb[:, :], func=AF.Identity, scale=pcol(P_PSCALE + c)),
                             [Bpb, Bprm], [By])
                        Yp.append((y, By))

                    def rms_group(Ys, nfeat, ci0):
                        srcs = []
                        for (y, By) in Ys:
                            sq, Bsq = fp.get()
                            S.op("pool", lambda e: e.tensor_tensor(out=sq[:], in0=y[:], in1=y[:], op=ALU.mult), [By], [Bsq])
                            srcs.append((sq[:], Bsq))
                        r, Br = stats_rinv(srcs, 1.0 / nfeat, RMS_EPS, fp, STb, BST)
                        for i, (y, By) in enumerate(Ys):
                            ci = ci0 + i
                            S.op("dve", lambda e: e.scalar_tensor_tensor(out=yn[:, ci, :], in0=y[:], scalar=pcol(P_GMIX + ci), in1=r[:],
                                                                         op0=ALU.mult, op1=ALU.mult), [By, Br, Bprm], [Byn[ci]])

                    rms_group(Yp, 256, 0)

                    chk("pool1")
                    Yc = []
                    Rbank = ps[4]
                    for hp in range(3):
                        jlist = [j_ for j_ in (3, 4, 2, 5, 1, 6, 0, 7) if (t > 0 or j_ >= 4)]
                        units = [(hh, j) for j in jlist for hh in range(2)]
                        cu = {}
                        firstC = [True, True]

                        def ca(u):
                            hh, j = units[u]
                            h = 2 * hp + hh
                            hb = hh * 64
                            jsl = sl if j >= 4 else 1 - sl
                            kcol = jsl * TT + (j % 4) * 128
                            qc_lo = max(0, 2 * j - 8)
                            qc_hi = min(7, 2 * j + 1)
                            qlo, qhi = qc_lo * 64, qc_hi * 64 + 64
                            N = qhi - qlo
                            Dd = qlo + 512 - 128 * j
                            masked = ((not full) and t == 1 and j < 4)
                            zb, Bzb = Z[u % 2], BZ[u % 2]
                            S.op("pe", lambda e: e.matmul(zb[:, 0:N], lhsT=kT_c[hb:hb + 64, hp, kcol:kcol + 128],
                                                          rhs=qT_c[hb:hb + 64, hp, qlo:qhi], start=True, stop=True),
                                 [BkTc[jsl][hp], BqTc[hp]], [Bzb])
                            if j <= 2:
                                ntab = 0
                            elif j == 3:
                                ntab = 128
                            else:
                                ntab = min(256, N)
                            P, BP = bp.get()
                            if ntab > 0:
                                tt_, Btt = fp.get()
                                g0 = P_GTAB + h * 256 + Dd
                                S.op("dve", lambda e: e.tensor_tensor(out=tt_[:, 0:ntab], in0=zb[:, 0:ntab], in1=prm[:, g0:g0 + ntab], op=ALU.add),
                                     [Bzb, Bprm], [Btt])
                                if masked:
                                    S.op("act", lambda e: e.activation(out=P[:, 0:ntab], in_=tt_[:, 0:ntab], func=AF.Exp, bias=pcol(P_CMASK + 1)),
                                         [Btt, Bprm], [BP])
                                else:
                                    S.op("act", lambda e: e.activation(out=P[:, 0:ntab], in_=tt_[:, 0:ntab], func=AF.Exp), [Btt], [BP])
                            if N > ntab:
                                bias_ap = small[:, h:h + 1] if masked else pcol(P_BIASC + h)
                                S.op("act", lambda e: e.activation(out=P[:, ntab:N], in_=zb[:, ntab:N], func=AF.Exp, bias=bias_ap),
                                     [Bzb, Bprm, Bsmall], [BP])
                            if j <= 3:
                                c0 = (2 * j + 1) * 64 - qlo
                                S.op("pool", lambda e: e.memset(P[0:64, c0:c0 + 64], 0.0), [], [BP])
                            else:
                                S.op("pool", lambda e: e.memset(P[64:128, 0:64], 0.0), [], [BP])
                            cu[u] = (hh, j, h, hb, jsl, qlo, qhi, N, P, BP)

                        def cb_(u):
                            hh, j, h, hb, jsl, qlo, qhi, N, P, BP = cu.pop(u)
                            S.op("pe", lambda e: e.matmul(Obank[hb:hb + 64, qlo:qhi], lhsT=v_c[:, jsl * 4 + j % 4, h * 64:(h + 1) * 64], rhs=P[:, 0:N],
                                                          start=firstC[hh], stop=(j == 7), skip_group_check=True),
                                 [Bvc[jsl][j % 4], BP], [BO[hh]])
                            S.op("pe", lambda e: e.matmul(Rbank[hb:hb + 64, qlo:qhi], lhsT=cstb[:, C_ONES:C_ONES + 64], rhs=P[:, 0:N],
                                                          start=firstC[hh], stop=(j == 7), skip_group_check=True),
                                 [Bcstb, BP], [BR2[hh]])
                            firstC[hh] = False

                        nu = len(units)
                        ca(0)
                        if nu > 1:
                            ca(1)
                        for u in range(nu):
                            cb_(u)
                            if u + 2 < nu:
                                ca(u + 2)
                                S.op("pe", lambda e: e.matmul(ps[0][:, 0:128], lhsT=cstb[:, C_ONES:C_ONES + 128], rhs=cstb[:, 0:128],
                                                              start=True, stop=True), [Bcstb], [Bps[0]], signal=False)
                        rec, Brec = fp.get()
                        S.op("dve", lambda e: e.reciprocal(out=rec[:], in_=Rbank[:, :]), BR2, [Brec])
                        y, By = ytile[hp], Bytile[hp]
                        S.op("dve", lambda e: e.tensor_tensor(out=y[:], in0=Obank[:, :], in1=rec[:], op=ALU.mult), BO + [Brec], [By])
                        Yc.append((y, By))
                    rms_group(Yc, 384, 2)

                    chk("chunk1")
                    Ys = []
                    nkt = 4 * t + 4
                    for hp in range(3):
                        tasks = [(hh, kt) for kt in range(nkt - 1, -1, -1) for hh in range(2)]
                        st = {}
                        firstA = [True, True]
                        firstO = [True, True]

                        def s1(i):
                            hh, kt = tasks[i]
                            hb = hh * 64
                            r = kt - 4 * t
                            c0 = 128 * r if r >= 0 else 0
                            N = TT - c0
                            masked = (not full) and kt < 4
                            zb, Bzb = Z[i % 2], BZ[i % 2]
                            S.op("pe", lambda e: e.matmul(zb[:, 0:N], lhsT=kT_s[hb:hb + 64, hp, kt * 128:(kt + 1) * 128],
                                                          rhs=qT_s[hb:hb + 64, hp, c0:TT], start=True, stop=True),
                                 [BkTs[kt // 4][hp], BqTs[hp]], [Bzb])
                            zs, Bzs = zp.get()
                            S.op("dve", lambda e: e.tensor_copy(out=zs[:, 0:N], in_=zb[:, 0:N]), [Bzb], [Bzs])
                            E, BE = fp.get()
                            S.op("act", lambda e: e.activation(out=E[:, 0:N], in_=zb[:, 0:N], func=AF.Exp), [Bzb], [BE])
                            SP, BSP = bp.get()
                            if masked:
                                S.op("act", lambda e: e.activation(out=SP[:, 0:N], in_=E[:, 0:N], func=AF.Ln, scale=pcol(P_CMASK), bias=1.0),
                                     [BE, Bprm], [BSP])
                            else:
                                S.op("act", lambda e: e.activation(out=SP[:, 0:N], in_=E[:, 0:N], func=AF.Ln, bias=1.0), [BE], [BSP])
                            if r >= 0:
                                S.op("pool", lambda e: e.tensor_tensor(out=SP[:, 0:128], in0=SP[:, 0:128], in1=cstb[:, C_MASK:C_MASK + 128], op=ALU.mult),
                                     [BSP, Bcstb], [BSP])
                            st[i] = dict(hh=hh, kt=kt, hb=hb, r=r, c0=c0, N=N, masked=masked, SP=SP, BSP=BSP, zs=zs, Bzs=Bzs)

                        def s2(i):
                            d = st[i]
                            hh, kt, hb, c0, N = d["hh"], d["kt"], d["hb"], d["c0"], d["N"]
                            zs, Bzs = d["zs"], d["Bzs"]
                            Ab, BAb = Abank[hh], BA[hh]
                            S.op("pe", lambda e: e.matmul(Ab[:, c0:TT], lhsT=cstb[:, C_TRI:C_TRI + 128], rhs=d["SP"][:, 0:N],
                                                          start=firstA[hh], stop=True, skip_group_check=True),
                                 [Bcstb, d["BSP"]], BAb)
                            firstA[hh] = False
                            S.op("dve", lambda e: e.tensor_tensor(out=zs[:, 0:N], in0=Ab[:, c0:TT], in1=zs[:, 0:N], op=ALU.subtract),
                                 BAb + [Bzs], [Bzs])
                            Wt, BW = bp.get()
                            if d["masked"]:
                                S.op("act", lambda e: e.activation(out=Wt[:, 0:N], in_=zs[:, 0:N], func=AF.Exp, scale=-1.0, bias=pcol(P_CMASK + 1)),
                                     [Bzs, Bprm], [BW])
                            else:
                                S.op("act", lambda e: e.activation(out=Wt[:, 0:N], in_=zs[:, 0:N], func=AF.Exp, scale=-1.0), [Bzs], [BW])
                            if d["r"] >= 0:
                                S.op("pool", lambda e: e.tensor_tensor(out=Wt[:, 0:128], in0=Wt[:, 0:128], in1=cstb[:, C_MASK:C_MASK + 128], op=ALU.mult),
                                     [BW, Bcstb], [BW])
                            d["W"], d["BW"] = Wt, BW

                        def filler(nf_):
                            for _ in range(nf_):
                                S.op("pe", lambda e: e.matmul(ps[0][:, 0:256], lhsT=cstb[:, C_ONES:C_ONES + 128], rhs=cstb[:, 0:256],
                                                              start=True, stop=True), [Bcstb], [Bps[0]], signal=False)

                        def s2b(i):
                            d = st[i]
                            hh, kt, c0, N = d["hh"], d["kt"], d["c0"], d["N"]
                            Ab, BAb = Abank[hh], BA[hh]
                            filler(NFILL)
                            if kt > 0:
                                S.op("pe", lambda e: e.matmul(Ab[:, c0:TT], lhsT=cstb[:, C_TRIU:C_TRIU + 128], rhs=d["SP"][:, 0:N],
                                                              start=False, stop=True, skip_group_check=True),
                                     [Bcstb, d["BSP"]], BAb)

                        def s3(i):
                            d = st.pop(i)
                            hh, kt, hb, c0, N = d["hh"], d["kt"], d["hb"], d["c0"], d["N"]
                            h = 2 * hp + hh
                            last = (kt == 0)
                            S.op("pe", lambda e: e.matmul(Obank[hb:hb + 64, c0:TT], lhsT=v_s[:, kt, h * 64:(h + 1) * 64], rhs=d["W"][:, 0:N],
                                                          start=firstO[hh], stop=last, skip_group_check=True),
                                 [Bvs[kt // 4][kt % 4], d["BW"]], [BO[hh]])
                            firstO[hh] = False
                            filler(NFILL)

                        n = len(tasks)
                        for i in range(-2, n + 2):
                            if 0 <= i < n:
                                s2(i)
                            if 0 <= i + 2 < n:
                                s1(i + 2)
                            if 0 <= i - 1 < n:
                                s2b(i - 1)
                            if 0 <= i - 2 < n:
                                s3(i - 2)
                        y, By = ytile[hp], Bytile[hp]
                        S.op("dve", lambda e: e.tensor_copy(out=y[:], in_=Obank[:, :]), BO, [By])
                        Ys.append((y, By))
                    rms_group(Ys, 384, 5)

                    if debug:
                        Bd = Buf()
                        S.dma("sp", dbg_yn[:, :, own_i * TT:(own_i + 1) * TT], yn[:], Bd, reads=Byn, writes=[Bd])

                    chk("sb1")
                    for dch in range(8):
                        pb, Bpb = pjbank()
                        for k in range(8):
                            S.op("pe", lambda e: e.matmul(pb[:, :], lhsT=w_out_bf[:, k, dch * 128:(dch + 1) * 128], rhs=yn[:, k, :],
                                                          start=(k == 0), stop=(k == 7)),
                                 [Bwout, Byn[k]], [Bpb], signal=(k == 7))
                        S.op("dve", lambda e: e.scalar_tensor_tensor(out=xres[:, dch, :], in0=xres[:, dch, :], scalar=ALPHA, in1=pb[:, :],
                                                                     op0=ALU.mult, op1=ALU.add), [Bxres[dch], Bxres_d, Bpb], [Bxres[dch]])
                    layer_norm(lambda d_: xres[:, d_, :], Bxres, P_LN1G, P_LN1B, fp, STb, BST, ps[2], Bps[2], lnMR, BlnMR)
                    S.dma("sp", x1s_v[:, :, own_i * TT:(own_i + 1) * TT], xres[:], Bxres_d, reads=Bxres + [Bxres_d], writes=[Bx1s[own_i]])
                    own_i += 1
                chk("mixer")
                S.barrier()
                for e_ in ENGS:
                    for b in Bx1s:
                        if not S.dead:
                            S._wait(e_, b.w)

            for hf in range(2 if full else 1):
                pfx2 = "L%dh%d_" % (li, hf)
                cb = hf * 4 * TT
                with ExitStack() as es:
                    sb = lambda name, shape, dtp: es.enter_context(nc.sbuf_tensor(pfx2 + name, shape, dtp))
                    x1bf = sb("x1bf", [128, 8, 4 * TT], BF16)
                    acc = sb("acc", [128, 8, 4 * TT], F32)
                    NWS = 2
                    wg_bf = [sb("wg_bf%d" % i, [128, 8, 512], BF16) for i in range(NWS)]
                    wu_bf = [sb("wu_bf%d" % i, [128, 8, 512], BF16) for i in range(NWS)]
                    wd_bf = [sb("wd_bf%d" % i, [128, 4, D], BF16) for i in range(NWS)]
                    hact = [sb("hact%d" % i, [128, 4, TT], BF16) for i in range(2)]
                    fp = TPool(nc, es, pfx2 + "ffp", [128, 512], F32, 6)
                    lnMR = [sb("flnMR%d" % i, [128, TT], F32) for i in range(2)]
                    BlnMR = [Buf(), Buf()]
                    Bx1bf = [Buf() for i in range(4)]
                    Bacc = [[Buf() for i in range(4)] for d_ in range(8)]
                    Bwg = [Buf() for i in range(NWS)]
                    Bwu = [Buf() for i in range(NWS)]
                    Bwd = [Buf() for i in range(NWS)]
                    Bhact = [[Buf() for c in range(4)] for i in range(2)]
                    for i in range(4):
                        S.dma("pool", x1bf[:, :, i * TT:(i + 1) * TT], x1s_v[:, :, cb + i * TT:cb + (i + 1) * TT], Bx1bf[i], reads=[Bx1s[4 * hf + i]], writes=[Bx1bf[i]])

                    if moe:
                        gB = [sb("gB%d" % i, [128, 4 * TT], F32) for i in range(2)]
                        BgB = [[Buf() for i in range(4)] for _ in range(2)]
                        gates = sb("gates", [128, 16, NE], F32)
                        Bgates = Buf("gates")
                        rt = sb("rt", [128, 8, NE], F32)
                        Brt = Buf("rt")
                        x1f = [sb("x1f0", [128, 8, 128], F32)] * 2
                        Bx1f = [Buf()] * 2
                        gsm = sb("gsm", [128, 64], F32)
                        Bgsm = Buf()
                        gexp = [sb("gexp%d" % i, [128, 128], F32) for i in range(2)]
                        Bgexp = [Buf(), Buf()]
                        S.dma("sp", rt[:], router.rearrange("(k p) e -> p k e", p=128), Brt, writes=[Brt])
                        RB, BRB = ps[7], Bps[7]
                        for s in range(16):
                            xf, Bxf = x1f[s % 2], Bx1f[s % 2]
                            S.dma("sp", xf[:], x1s_v[:, :, cb + s * 128:cb + (s + 1) * 128], Bxf, reads=[Bx1s[4 * hf + s // 4]], writes=[Bxf])
                            for k in range(8):
                                S.op("pe", lambda e: e.matmul(RB[:, 0:NE], lhsT=xf[:, k, :], rhs=rt[:, k, :], start=(k == 0), stop=(k == 7)),
                                     [Bxf, Brt], [BRB])
                            lg = gsm[:, 0:8]
                            S.op("act", lambda e: e.activation(out=lg, in_=RB[:, 0:NE], func=AF.Copy), [BRB], [Bgsm])
                            m8 = gsm[:, 8:16]
                            S.op("dve", lambda e: e.max(out=m8, in_=lg), [Bgsm], [Bgsm])
                            dd = gsm[:, 16:17]
                            S.op("dve", lambda e: e.tensor_tensor(out=dd, in0=gsm[:, 9:10], in1=gsm[:, 8:9], op=ALU.subtract), [Bgsm], [Bgsm])
                            ee = gsm[:, 17:18]
                            S.op("act", lambda e: e.activation(out=ee, in_=dd, func=AF.Exp), [Bgsm], [Bgsm])
                            den = gsm[:, 18:19]
                            S.op("dve", lambda e: e.tensor_scalar(out=den, in0=ee, scalar1=1.0, scalar2=None, op0=ALU.add), [Bgsm], [Bgsm])
                            w1 = gsm[:, 19:20]
                            S.op("dve", lambda e: e.reciprocal(out=w1, in_=den), [Bgsm], [Bgsm])
                            w2 = gsm[:, 20:21]
                            S.op("dve", lambda e: e.tensor_tensor(out=w2, in0=ee, in1=w1, op=ALU.mult), [Bgsm], [Bgsm])
                            m1 = gsm[:, 24:32]
                            S.op("dve", lambda e: e.tensor_scalar(out=m1, in0=lg, scalar1=gsm[:, 8:9], scalar2=w1, op0=ALU.is_equal, op1=ALU.mult),
                                 [Bgsm], [Bgsm])
                            m2 = gsm[:, 32:40]
                            S.op("dve", lambda e: e.tensor_scalar(out=m2, in0=lg, scalar1=gsm[:, 9:10], scalar2=w2, op0=ALU.is_equal, op1=ALU.mult),
                                 [Bgsm], [Bgsm])
                            S.op("dve", lambda e: e.tensor_tensor(out=gates[:, s, :], in0=m1, in1=m2, op=ALU.add), [Bgsm], [Bgates])

                    GU = [(ps[0], Bps[0], ps[1], Bps[1]), (ps[2], Bps[2], ps[3], Bps[3])]
                    OBk = [(ps[4], Bps[4]), (ps[5], Bps[5]), (ps[6], Bps[6])]
                    groups = [(0, 4), (4, 4), (8, 4), (12, 4), (16, 4), (20, 2)]
                    ws_i = 0
                    gu_i = 0
                    ob_i = 0
                    ha_i = 0
                    first_acc = True
                    for ex in range(NE if moe else 1):
                        if moe:
                            gb, Bgb = gB[ex % 2], BgB[ex % 2]
                            for s in range(16):
                                ge, Bge = gexp[s % 2], Bgexp[s % 2]
                                S.op("dve", lambda e: e.tensor_scalar(out=ge[:], in0=onesf, scalar1=gates[:, s, ex:ex + 1], scalar2=None, op0=ALU.mult),
                                     [Bcstf, Bgates], [Bge])
                                S.op("pe", lambda e: e.matmul(ps[7][:, (s % 4) * 128:(s % 4 + 1) * 128], lhsT=ge[:], rhs=identf, start=True, stop=True),
                                     [Bge, Bcstf], [Bps[7]])
                                if s % 4 == 3:
                                    i4 = s // 4
                                    S.op("act", lambda e: e.activation(out=gb[:, i4 * TT:(i4 + 1) * TT], in_=ps[7][:, :], func=AF.Copy), [Bps[7]], [Bgb[i4]])
                        for (f0, nf) in groups:
                            w_i = ws_i % NWS
                            ws_i += 1
                            S.dma("pool", wg_bf[w_i][:, :, 0:nf * 128], wg[ex, 131072 * f0:131072 * (f0 + nf)].rearrange("(p k f) -> p k f", p=128, k=8),
                                  Bwg[w_i], writes=[Bwg[w_i]])
                            S.dma("pool", wu_bf[w_i][:, :, 0:nf * 128], wu[ex, 131072 * f0:131072 * (f0 + nf)].rearrange("(p k f) -> p k f", p=128, k=8),
                                  Bwu[w_i], writes=[Bwu[w_i]])
                            S.dma("pool", wd_bf[w_i][:, 0:nf, :], wd[ex, 131072 * f0:131072 * (f0 + nf)].rearrange("(p c d) -> p c d", p=128, c=nf),
                                  Bwd[w_i], writes=[Bwd[w_i]])
                            for i in range(4):
                                ha, Bha = hact[ha_i % 2], Bhact[ha_i % 2]
                                ha_i += 1
                                for fc in range(nf):
                                    G, BG, Ub, BU = GU[gu_i % 2]
                                    gu_i += 1
                                    for k in range(8):
                                        S.op("pe", lambda e: e.matmul(G[:, :], lhsT=wg_bf[w_i][:, k, fc * 128:(fc + 1) * 128], rhs=x1bf[:, k, i * TT:(i + 1) * TT],
                                                                      start=(k == 0), stop=(k == 7)), [Bwg[w_i], Bx1bf[i]], [BG], signal=(k == 7))
                                    for k in range(8):
                                        S.op("pe", lambda e: e.matmul(Ub[:, :], lhsT=wu_bf[w_i][:, k, fc * 128:(fc + 1) * 128], rhs=x1bf[:, k, i * TT:(i + 1) * TT],
                                                                      start=(k == 0), stop=(k == 7)), [Bwu[w_i], Bx1bf[i]], [BU], signal=(k == 7))
                                    sg, Bsg = fp.get()
                                    S.op("act", lambda e: e.activation(out=sg[:], in_=G[:, :], func=AF.Silu), [BG], [Bsg])
                                    if moe:
                                        S.op("dve", lambda e: e.tensor_tensor(out=sg[:], in0=sg[:], in1=Ub[:, :], op=ALU.mult), [Bsg, BU], [Bsg])
                                        S.op("dve", lambda e: e.tensor_tensor(out=ha[:, fc, :], in0=sg[:], in1=gb[:, i * TT:(i + 1) * TT], op=ALU.mult),
                                             [Bsg, Bgb[i]], [Bha[fc]])
                                    else:
                                        S.op("dve", lambda e: e.tensor_tensor(out=ha[:, fc, :], in0=sg[:], in1=Ub[:, :], op=ALU.mult), [Bsg, BU], [Bha[fc]])
                                for dch in range(8):
                                    ob, Bob = OBk[ob_i % 3]
                                    ob_i += 1
                                    for fc in range(nf):
                                        S.op("pe", lambda e: e.matmul(ob[:, :], lhsT=wd_bf[w_i][:, fc, dch * 128:(dch + 1) * 128], rhs=ha[:, fc, :],
                                                                      start=(fc == 0), stop=(fc == nf - 1)), [Bwd[w_i], Bha[fc]], [Bob], signal=(fc == nf - 1))
                                    if first_acc:
                                        S.op("act", lambda e: e.activation(out=acc[:, dch, i * TT:(i + 1) * TT], in_=ob[:, :], func=AF.Copy), [Bob], [Bacc[dch][i]])
                                    else:
                                        S.op("dve", lambda e: e.tensor_tensor(out=acc[:, dch, i * TT:(i + 1) * TT], in0=acc[:, dch, i * TT:(i + 1) * TT],
                                                                              in1=ob[:, :], op=ALU.add), [Bacc[dch][i], Bob], [Bacc[dch][i]])
                            first_acc = False

                    for i in range(4):
                        for dch in range(8):
                            xt_, Bxt = fp.get()
                            S.dma("sp", xt_[:], x1s[dch * 128:(dch + 1) * 128, cb + i * TT:cb + (i + 1) * TT], Bxt, reads=[Bx1s[4 * hf + i]], writes=[Bxt])
                            S.op("dve", lambda e: e.scalar_tensor_tensor(out=acc[:, dch, i * TT:(i + 1) * TT], in0=xt_[:], scalar=ALPHA,
                                                                         in1=acc[:, dch, i * TT:(i + 1) * TT], op0=ALU.mult, op1=ALU.add),
                                 [Bxt, Bacc[dch][i]], [Bacc[dch][i]])
                        Bv = [Bacc[d_][i] for d_ in range(8)]
                        layer_norm(lambda d_: acc[:, d_, i * TT:(i + 1) * TT], Bv, P_LN2G, P_LN2B, fp, ps[7], Bps[7], ps[6], Bps[6], lnMR, BlnMR)
                        Bo_sb = Buf()
                        S.dma("sp", dst_v[:, :, cb + i * TT:cb + (i + 1) * TT], acc[:, :, i * TT:(i + 1) * TT], Bo_sb, reads=Bv, writes=[Bdst[4 * hf + i]])
                S.barrier()
                for e_ in ENGS:
                    for b in Bdst[4 * hf:4 * hf + 4]:
                        if not S.dead:
                            S._wait(e_, b.w)

        Bxin = [Buf() for t in range(NT)]
        Bx2s = [Buf("x2s%d" % t) for t in range(NT)]
        Bxs2 = [Buf("xs2%d" % t) for t in range(NT)]
        emit_layer(0, False, True, xT_v, Bxin, x2s_v, Bx2s)
        with ExitStack() as es:
            S.dma("sp", prm[:], prm_all[1], Bprm, writes=[Bprm])
            bl = TPool(nc, es, "bl", [128, 8, TT], F32, 4)
            for t in range(NT):
                a1, Ba1 = bl.get()
                S.dma("sp", a1[:], x2s_v[:, :, t * TT:(t + 1) * TT], Ba1, reads=[Bx2s[t]], writes=[Ba1])
                S.op("dve", lambda e: e.tensor_scalar(out=a1[:], in0=a1[:], scalar1=pcol(P_CMASK), scalar2=None, op0=ALU.mult), [Ba1, Bprm], [Ba1])
                if t > 0:
                    a0, Ba0 = bl.get()
                    S.dma("sp", a0[:], x2s_v[:, :, (t - 1) * TT:t * TT], Ba0, reads=[Bx2s[t - 1]], writes=[Ba0])
                    S.op("dve", lambda e: e.scalar_tensor_tensor(out=a1[:], in0=a0[:], scalar=pcol(P_M0), in1=a1[:], op0=ALU.mult, op1=ALU.add),
                         [Ba0, Ba1, Bprm], [Ba1])
                S.dma("sp", xs2_v[:, :, t * TT:(t + 1) * TT], a1[:], Ba1, reads=[Ba1], writes=[Bxs2[t]])
            S.barrier()
            for e_ in ENGS:
                for b in Bxs2:
                    S._wait(e_, b.w)
        emit_layer(1, True, False, xs2_v, Bxs2, outT_v, Bout + [Buf() for _ in range(4)])
        S.finish(Bout)
        print("built fused: ops=%d waits=%d dma_sems=%d" % (S.nop, S.nwait, S.ndsem))
    return nc


def _consts():
    c = np.zeros((128, NCST), np.float32)
    j = np.arange(128)[:, None]
    k = np.arange(128)[None, :]
    c[:, C_TRI:C_TRI + 128] = (j >= k)
    c[:, C_TRIU:C_TRIU + 128] = (j < k)
    c[:, C_ONES:C_ONES + 128] = 1.0
    c[:, C_MASK:C_MASK + 128] = (j < k)
    c[:, C_IDENT:C_IDENT + 128] = np.eye(128)
    return c


def _cols(v):
    return np.ascontiguousarray(v.reshape(8, 128).T)


def _prm(j, li, full, pool_scale, g_mix, ln1_g, ln1_b, ln2_g, ln2_b, rel_bias):
    p = np.zeros((128, NPRM), np.float32)
    p[:, P_PSCALE:P_PSCALE + 2] = pool_scale[li].reshape(2, 128).T
    p[:, P_GMIX:P_GMIX + 8] = _cols(g_mix[li])
    p[:, P_LN1G:P_LN1G + 8] = _cols(ln1_g[li])
    p[:, P_LN1B:P_LN1B + 8] = _cols(ln1_b[li])
    p[:, P_LN2G:P_LN2G + 8] = _cols(ln2_g[li])
    p[:, P_LN2B:P_LN2B + 8] = _cols(ln2_b[li])
    rb = rel_bias[li]
    p[:, P_BIASC:P_BIASC + 6] = rb[:, 256][None, :]
    real = full or j == 1
    p[:, P_CMASK] = 1.0 if real else 0.0
    p[:, P_CMASK + 1] = 0.0 if real else -100.0
    p[:, P_M0] = 0.0 if j == 1 else 1.0
    wins = np.array([2, 4, 8, 16], np.float32)
    tau = np.arange(16, dtype=np.float32)
    seq_start = full or j == 0
    for c in range(2):
        for half in range(2):
            w = wins[c * 2 + half]
            if seq_start:
                ic = 1.0 / np.minimum(tau + 1.0, w)
            else:
                ic = np.full(16, 1.0 / w, np.float32)
            p[half * 64:(half + 1) * 64, P_ICNT + c * 16:P_ICNT + c * 16 + 16] = ic[None, :]
    pp = np.arange(128)[:, None]
    uu = np.arange(256)[None, :]
    idx = np.clip(uu - pp, -128, 128) + 128
    for h in range(6):
        p[:, P_GTAB + h * 256:P_GTAB + (h + 1) * 256] = rb[h][idx]
    return p


_GROUPS = [(0, 4), (4, 4), (8, 4), (12, 4), (16, 4), (20, 2)]


def _relayout_gu(w):
    E = w.shape[0]
    out = np.empty((E, D * DFF), np.float32)
    for e in range(E):
        w3 = w[e].reshape(8, 128, DFF)
        pos = 0
        for (f0, nf) in _GROUPS:
            blk = w3[:, :, f0 * 128:(f0 + nf) * 128].transpose(1, 0, 2)
            out[e, pos:pos + blk.size] = blk.reshape(-1)
            pos += blk.size
    return out


def _relayout_d(w):
    E = w.shape[0]
    out = np.empty((E, DFF * D), np.float32)
    for e in range(E):
        pos = 0
        for (f0, nf) in _GROUPS:
            blk = w[e][f0 * 128:(f0 + nf) * 128].reshape(nf, 128, D).transpose(1, 0, 2)
            out[e, pos:pos + blk.size] = blk.reshape(-1)
            pos += blk.size
    return out


_NC = []


def kernel(**inputs):
    inp = {k: np.asarray(v) for k, v in inputs.items()}
    x = np.ascontiguousarray(inp["x"], dtype=np.float32)
    if not _NC:
        _NC.append(build_fused())
    nc = _NC[0]
    cst = _consts()
    pwbd = np.zeros((2, 128, 2, 128), np.float32)
    for li in range(2):
        for c in range(2):
            pwbd[li, 0:64, c, 0:64] = inp["pool_w"][li][2 * c]
            pwbd[li, 64:128, c, 64:128] = inp["pool_w"][li][2 * c + 1]
    wg_r, wu_r, wd_r = _relayout_gu(inp["ffn_wg"]), _relayout_gu(inp["ffn_wu"]), _relayout_d(inp["ffn_wd"])
    mwg_r, mwu_r, mwd_r = _relayout_gu(inp["moe_wg"][0]), _relayout_gu(inp["moe_wu"][0]), _relayout_d(inp["moe_wd"][0])
    in_maps = []
    for core in range(8):
        b, j = core // 2, core % 2
        prm = np.stack([_prm(j, li, li == 0, inp["pool_scale"], inp["g_mix"], inp["ln1_g"], inp["ln1_b"], inp["ln2_g"],
                             inp["ln2_b"], inp["rel_bias"]) for li in range(2)])
        in_maps.append({"xT": np.ascontiguousarray(x[b].T), "w_in": inp["w_in"], "w_out": inp["w_out"], "pwbd": pwbd,
                        "prm": prm, "cst": cst, "wg": wg_r, "wu": wu_r, "wd": wd_r, "mwg": mwg_r, "mwu": mwu_r, "mwd": mwd_r,
                        "router": inp["moe_router"][0]})
    res = run_bass_kernel_spmd(nc, in_maps, core_ids=list(range(8)))
    out = np.empty_like(x)
    for core in range(8):
        b, j = core // 2, core % 2
        o = res.results[core]["outT"]
        for i in range(4):
            g = 2 * i + j
            out[b, g * TT:(g + 1) * TT, :] = o[:, i * TT:(i + 1) * TT].T
    return out
```

```python
import numpy as np
from contextlib import ExitStack
import concourse.bass as bass
import concourse.mybir as mybir
from concourse.bass_utils import run_bass_kernel_spmd

F32 = mybir.dt.float32
BF16 = mybir.dt.bfloat16
AF = mybir.ActivationFunctionType
ALU = mybir.AluOpType

ENGS = ("pe", "act", "dve", "pool", "sp")

D = 1024
SEQ = 4096
TT = 512
NT = 8
DFF = 2816
NE = 8
ALPHA = 4.0 ** 0.25
LN_EPS = 1e-5
RMS_EPS = 1e-6
OWN = (1, 3, 5, 7)
NFILL = 2

P_PSCALE = 0
P_GMIX = 2
P_LN1G = 10
P_LN1B = 18
P_LN2G = 26
P_LN2B = 34
P_BIASC = 42
P_CMASK = 48
P_ICNT = 50
P_GTAB = 82
P_M0 = 82 + 6 * 256
NPRM = P_M0 + 1
C_TRI = 0
C_TRIU = 128
C_ONES = 256
C_MASK = 384
C_IDENT = 512
NCST = 640


class Buf:
    __slots__ = ("name", "w", "rd", "dsem", "dcnt", "dkey", "excl")

    def __init__(self, name="", excl=False):
        self.name = name
        self.excl = excl
        self.w = None
        self.rd = []
        self.dsem = None
        self.dcnt = 0
        self.dkey = None


class Sched:
    def __init__(self, nc, es):
        self.nc = nc
        self.es = es
        self.eng = dict(pe=nc.tensor, act=nc.scalar, dve=nc.vector, pool=nc.gpsimd, sp=nc.sync)
        self.semh = {}
        for e in ENGS:
            self.semh[e] = es.enter_context(nc.semaphore("s_" + e))
        self.cnt = {e: 0 for e in ENGS}
        self.pending = {e: False for e in ENGS}
        self.seen = {e: {} for e in ENGS}
        self.snap = {}
        self.nwait = 0
        self.nop = 0
        self.ndsem = 0
        self.dead = False
        self.dbufs = []

    def _wait(self, e, t):
        if t is None:
            return
        key, val = t
        if key == e and e == "pe":
            return
        sn = self.seen[e]
        if sn.get(key, 0) >= val:
            return
        if key == e and self.cnt[e] < val:
            raise RuntimeError("self-wait on pending ticket " + e)
        self.eng[e].wait_ge(self.semh[key], val)
        self.nwait += 1
        sn[key] = val
        s = self.snap.get(t)
        if s is not None:
            for k, v in s:
                if sn.get(k, 0) < v:
                    sn[k] = v

    def _deps(self, e, reads, writes):
        for b in reads:
            self._wait(e, b.w)
        for b in writes:
            self._wait(e, b.w)
            for t in b.rd:
                self._wait(e, t)

    def _commit(self, t, reads, writes):
        for b in reads:
            b.rd.append(t)
            if len(b.rd) > 64:
                last = {}
                for k, v in b.rd:
                    if last.get(k, 0) < v:
                        last[k] = v
                b.rd = list(last.items())
        for b in writes:
            b.w = t
            b.rd = []

    def op(self, e, fn, reads=(), writes=(), signal=True):
        if self.dead:
            return None
        ex = [b for b in reads if b.excl]
        if ex:
            writes = list(writes) + ex
        self._deps(e, reads, writes)
        ins = fn(self.eng[e])
        self.nop += 1
        if signal:
            ins.then_inc(self.semh[e], 1)
            self.cnt[e] += 1
            t = (e, self.cnt[e])
            self.pending[e] = False
            sn = self.seen[e]
            self.snap[t] = tuple((k, sn.get(k, 0)) for k in ENGS if k != e and sn.get(k, 0) > 0)
        else:
            t = (e, self.cnt[e] + 1)
            self.pending[e] = True
        self._commit(t, reads, writes)
        return t

    def dma(self, q, out, in_, sb, reads=(), writes=(), **kw):
        if self.dead:
            return None
        self._deps(q, reads, writes)
        if sb.dsem is None:
            sb.dsem = self.es.enter_context(self.nc.semaphore("d%d" % self.ndsem))
            sb.dkey = ("d", self.ndsem)
            self.semh[sb.dkey] = sb.dsem
            self.ndsem += 1
            self.dbufs.append(sb)
        ins = self.eng[q].dma_start(out=out, in_=in_, **kw)
        ins.then_inc(sb.dsem, 16)
        sb.dcnt += 16
        t = (sb.dkey, sb.dcnt)
        self._commit(t, reads, writes)
        return t

    def barrier(self):
        if self.dead:
            return
        ts = [(e, self.cnt[e]) for e in ENGS if self.cnt[e] > 0]
        for e in ENGS:
            for t in ts:
                if t[0] != e:
                    self._wait(e, t)

    def finish(self, bufs):
        for b in self.dbufs:
            self._wait("sp", (b.dkey, b.dcnt))
        for b in bufs:
            self._wait("sp", b.w)
            for t in b.rd:
                self._wait("sp", t)
        for e in ENGS:
            assert not self.pending[e], e


class TPool:
    def __init__(self, nc, es, name, shape, dtype, n):
        self.t = [es.enter_context(nc.sbuf_tensor("%s%d" % (name, i), shape, dtype)) for i in range(n)]
        self.b = [Buf("%s%d" % (name, i)) for i in range(n)]
        self.i = 0

    def get(self):
        i = self.i
        self.i = (i + 1) % len(self.t)
        return self.t[i], self.b[i]


class _Stop(Exception):
    pass


def build_fused(debug=False, stage=None):
    nc = bass.Bass("TRN2", target_bir_lowering=False)

    def chk(name):
        if stage == name:
            S.dead = True
    dt = nc.dram_tensor
    xT = dt("xT", [D, SEQ], F32, kind="ExternalInput").ap()
    w_in_all = dt("w_in", [2, D, 2560], F32, kind="ExternalInput").ap()
    w_out_all = dt("w_out", [2, D, D], F32, kind="ExternalInput").ap()
    pwbd_all = dt("pwbd", [2, 128, 2, 128], F32, kind="ExternalInput").ap()
    prm_all = dt("prm", [2, 128, NPRM], F32, kind="ExternalInput").ap()
    cst_d = dt("cst", [128, NCST], F32, kind="ExternalInput").ap()
    wg_d = dt("wg", [1, D * DFF], F32, kind="ExternalInput").ap()
    wu_d = dt("wu", [1, D * DFF], F32, kind="ExternalInput").ap()
    wd_d = dt("wd", [1, DFF * D], F32, kind="ExternalInput").ap()
    wg_m = dt("mwg", [NE, D * DFF], F32, kind="ExternalInput").ap()
    wu_m = dt("mwu", [NE, D * DFF], F32, kind="ExternalInput").ap()
    wd_m = dt("mwd", [NE, DFF * D], F32, kind="ExternalInput").ap()
    router = dt("router", [D, NE], F32, kind="ExternalInput").ap()
    outT = dt("outT", [D, 4 * TT], F32, kind="ExternalOutput").ap()
    x1s = dt("x1s", [D, SEQ], F32, kind="Internal").ap()
    x2s = dt("x2s", [D, SEQ], F32, kind="Internal").ap()
    xs2 = dt("xs2", [D, SEQ], F32, kind="Internal").ap()

    xT_v = xT.rearrange("(k p) n -> p k n", p=128)
    outT_v = outT.rearrange("(k p) n -> p k n", p=128)
    x1s_v = x1s.rearrange("(k p) n -> p k n", p=128)
    x2s_v = x2s.rearrange("(k p) n -> p k n", p=128)
    xs2_v = xs2.rearrange("(k p) n -> p k n", p=128)

    with ExitStack() as es0:
        S = Sched(nc, es0)
        ps = [es0.enter_context(nc.psum_tensor("ps%d" % i, [128, 512], F32)) for i in range(8)]
        Bps = [Buf("ps%d" % i, True) for i in range(8)]
        prm = es0.enter_context(nc.sbuf_tensor("prm_sb", [128, NPRM], F32))
        cstf = es0.enter_context(nc.sbuf_tensor("cstf", [128, 256], F32))
        cstb = es0.enter_context(nc.sbuf_tensor("cstb", [128, NCST], BF16))
        Bprm, Bcstf, Bcstb = Buf("prm"), Buf("cstf"), Buf("cstb")
        S.dma("sp", cstf[:, 0:128], cst_d[:, C_ONES:C_ONES + 128], Bcstf, writes=[Bcstf])
        S.dma("sp", cstf[:, 128:256], cst_d[:, C_IDENT:C_IDENT + 128], Bcstf, writes=[Bcstf])
        S.dma("pool", cstb[:], cst_d, Bcstb, writes=[Bcstb])
        Bout = [Buf("out%d" % i) for i in range(4)]
        onesf = cstf[:, 0:128]
        identf = cstf[:, 128:256]

        def pcol(c, n=1):
            return prm[:, c:c + n]

        def stats_rinv(srcs, scale, eps, fp, bank, Bbank):
            n = len(srcs)
            for i, (a, b) in enumerate(srcs):
                S.op("pe", lambda e: e.matmul(bank[:, :], lhsT=onesf, rhs=a, start=(i == 0), stop=(i == n - 1)),
                     [Bcstf, b], [Bbank])
            l, Bl = fp.get()
            S.op("act", lambda e: e.activation(out=l[:], in_=bank[:, :], func=AF.Ln, scale=scale, bias=eps), [Bbank], [Bl])
            r, Br = fp.get()
            S.op("act", lambda e: e.activation(out=r[:], in_=l[:], func=AF.Exp, scale=-0.5), [Bl], [Br])
            return r, Br

        def layer_norm(vch, Bv, gcol, bcol, fp, bankA, BbankA, bankB, BbankB, MR, BMR):
            sqs = []
            for d in range(8):
                S.op("pe", lambda e: e.matmul(bankA[:, :], lhsT=onesf, rhs=vch(d), start=(d == 0), stop=(d == 7)),
                     [Bcstf, Bv[d]], [BbankA])
            for d in range(8):
                sq, Bsq = fp.get()
                S.op("pool", lambda e: e.tensor_tensor(out=sq[:], in0=vch(d), in1=vch(d), op=ALU.mult), [Bv[d]], [Bsq])
                S.op("pe", lambda e: e.matmul(bankB[:, :], lhsT=onesf, rhs=sq[:], start=(d == 0), stop=(d == 7)),
                     [Bcstf, Bsq], [BbankB])
            M, BM = MR[0], BMR[0]
            S.op("act", lambda e: e.activation(out=M[:], in_=bankA[:, :], func=AF.Identity, scale=1.0 / D), [BbankA], [BM])
            msq, Bmsq = fp.get()
            S.op("dve", lambda e: e.tensor_tensor(out=msq[:], in0=M[:], in1=M[:], op=ALU.mult), [BM], [Bmsq])
            V, BV = fp.get()
            S.op("dve", lambda e: e.scalar_tensor_tensor(out=V[:], in0=bankB[:, :], scalar=1.0 / D, in1=msq[:],
                                                         op0=ALU.mult, op1=ALU.subtract), [BbankB, Bmsq], [BV])
            L, BL = fp.get()
            S.op("act", lambda e: e.activation(out=L[:], in_=V[:], func=AF.Ln, bias=LN_EPS), [BV], [BL])
            Rr, BR = MR[1], BMR[1]
            S.op("act", lambda e: e.activation(out=Rr[:], in_=L[:], func=AF.Exp, scale=-0.5), [BL], [BR])
            for d in range(8):
                t1, Bt1 = fp.get()
                S.op("pool", lambda e: e.tensor_tensor(out=t1[:], in0=vch(d), in1=M[:], op=ALU.subtract), [Bv[d], BM], [Bt1])
                S.op("dve", lambda e: e.tensor_tensor(out=t1[:], in0=t1[:], in1=Rr[:], op=ALU.mult), [Bt1, BR], [Bt1])
                S.op("act", lambda e: e.activation(out=vch(d), in_=t1[:], func=AF.Identity,
                                                   scale=pcol(gcol + d), bias=pcol(bcol + d)), [Bt1, Bprm], [Bv[d]])

        def emit_layer(li, moe, full, xsrc_v, Bxsrc, dst_v, Bdst):
            pfx = "L%d_" % li
            w_in_v = w_in_all[li].rearrange("(k p) f -> p k f", p=128)
            w_out_v = w_out_all[li].rearrange("(k p) f -> p k f", p=128)
            pwbd = pwbd_all[li]
            wg, wu, wd = (wg_m, wu_m, wd_m) if moe else (wg_d, wu_d, wd_d)
            S.dma("sp", prm[:], prm_all[li], Bprm, writes=[Bprm])
            Bx1s = [Buf("x1s%d" % i) for i in range(8)]
            with ExitStack() as es:
                sb = lambda name, shape, dtp: es.enter_context(nc.sbuf_tensor(pfx + name, shape, dtp))
                w_in_bf = sb("w_in_bf", [128, 8, 2560], BF16)
                w_out_bf = sb("w_out_bf", [128, 8, D], BF16)
                pw_bf = sb("pw_bf", [128, 2, 128], BF16)
                kT_s = sb("kT_s", [128, 3, SEQ], BF16)
                v_s = sb("v_s", [128, 32, 384], BF16)
                kT_c = sb("kT_c", [128, 3, 2 * TT], BF16)
                v_c = sb("v_c", [128, 8, 384], BF16)
                qT_c = sb("qT_c", [128, 3, TT], BF16)
                qT_s = sb("qT_s", [128, 3, TT], BF16)
                xbf = [sb("xbf0", [128, 8, TT], BF16)] * 2
                ubuf = [sb("ubuf%d" % i, [128, 2, 16 + TT], F32) for i in range(2)]
                ytile = [sb("ytile%d" % i, [128, TT], F32) for i in range(3)]
                Bytile = [Buf() for i in range(3)]
                lnMR = [sb("lnMR%d" % i, [128, TT], F32) for i in range(2)]
                BlnMR = [Buf(), Buf()]
                yn = sb("yn", [128, 8, TT], BF16)
                xres = sb("xres", [128, 8, TT], F32)
                small = sb("small", [128, 32], F32)
                fp = TPool(nc, es, pfx + "fp", [128, 512], F32, 5)
                zp = TPool(nc, es, pfx + "zp", [128, 16 + TT], F32, 4)
                bp = TPool(nc, es, pfx + "bp", [128, 512], BF16, 8)

                Bwin = [Buf("win%d" % i) for i in range(4)]
                Bwout, Bpw = Buf("wout"), Buf("pw")
                BkTs = [[Buf() for c in range(3)] for t in range(NT)]
                Bvs = [[Buf() for s in range(4)] for t in range(NT)]
                BkTc = [[Buf() for c in range(3)] for sl in range(2)]
                Bvc = [[Buf() for s in range(4)] for sl in range(2)]
                BqTc = [Buf() for c in range(3)]
                BqTs = [Buf() for c in range(3)]
                BqTn = [Buf() for c in range(3)]
                Bxbf = [Buf("xbf0")] * 2
                Bu = [Buf("u0"), Buf("u1")]
                Bpt = [Buf("pt0"), Buf("pt1")]
                Byn = [Buf() for c in range(8)]
                Bxres = [Buf() for c in range(8)]
                Bxres_d = Buf("xres_d")
                Bsmall = Buf("small")

                for c in range(4):
                    S.dma("pool", w_in_bf[:, :, c * 640:(c + 1) * 640], w_in_v[:, :, c * 640:(c + 1) * 640], Bwin[c], writes=[Bwin[c]])
                S.dma("pool", pw_bf[:], pwbd, Bpw, writes=[Bpw])
                S.op("dve", lambda e: e.tensor_scalar(out=small[:, 0:6], in0=pcol(P_BIASC, 6), scalar1=pcol(P_CMASK + 1),
                                                      scalar2=None, op0=ALU.add), [Bprm], [Bsmall])
                S.op("dve", lambda e: e.memset(ubuf[0][:, :, 0:16], 0.0), [], [Bu[0]])

                pj_i = [0]

                def pjbank():
                    i = pj_i[0]
                    pj_i[0] = 1 - i
                    return ps[i], Bps[i]

                ev_i = [0]

                def evac(out, in_, reads, writes, scale=None):
                    ev_i[0] ^= 1
                    if ev_i[0]:
                        if scale is None:
                            S.op("act", lambda e: e.activation(out=out, in_=in_, func=AF.Copy), reads, writes)
                        else:
                            S.op("act", lambda e: e.activation(out=out, in_=in_, func=AF.Identity, scale=scale), reads, writes)
                    else:
                        if scale is None:
                            S.op("dve", lambda e: e.tensor_copy(out=out, in_=in_), reads, writes)
                        else:
                            S.op("dve", lambda e: e.tensor_scalar(out=out, in0=in_, scalar1=scale, scalar2=None, op0=ALU.mult),
                                 reads, writes)

                Z = [ps[2], ps[3]]
                BZ = [Bps[2], Bps[3]]
                Abank = [ps[4], ps[5]]
                B4 = [Buf("ps4lo", True), Buf("ps4hi", True)]
                BA = [B4, [Bps[5]]]
                Obank = ps[6]
                BO = [Buf("O_lo", True), Buf("O_hi", True)]
                STb, BST = ps[7], Bps[7]
                BR2 = B4

                own_i = 0
                chk("setup")
                for t in range(NT):
                    if t == 1:
                        chk("proj0")
                    if t == 2:
                        chk("own1")
                    own = full or (t in OWN)
                    first_own = (t == (0 if full else 1))
                    sl = t % 2
                    xb, Bxb = xbf[sl], Bxbf[sl]
                    if t == 0:
                        S.dma("pool", xb[:], xsrc_v[:, :, 0:TT], Bxb, reads=[Bxsrc[0]], writes=[Bxb])
                        S.dma("pool", w_out_bf[:], w_out_v, Bwout, writes=[Bwout])
                    if own:
                        S.dma("sp", xres[:], xsrc_v[:, :, t * TT:(t + 1) * TT], Bxres_d, reads=[Bxsrc[t]], writes=Bxres + [Bxres_d])

                    def proj_fm(col, out_fn):
                        pb, Bpb = pjbank()
                        for k in range(8):
                            S.op("pe", lambda e: e.matmul(pb[:, :], lhsT=w_in_bf[:, k, col:col + 128], rhs=xb[:, k, :],
                                                          start=(k == 0), stop=(k == 7)),
                                 [Bwin[col // 640], Bxb], [Bpb], signal=(k == 7))
                        out_fn(pb, Bpb)

                    if t == 1:
                        chk("t1a")
                    U = ubuf[sl]
                    for c in range(2):
                        proj_fm(c * 128, lambda pb, Bpb: evac(U[:, c, 16:16 + TT], pb[:, :], [Bpb], [Bu[sl]]))
                    if t < NT - 1:
                        S.op("dve", lambda e: e.tensor_copy(out=ubuf[1 - sl][:, :, 0:16], in_=U[:, :, TT:TT + 16]), [Bu[sl]], [Bu[1 - sl]])
                    for c in range(3):
                        proj_fm(640 + c * 128, lambda pb, Bpb: evac(kT_c[:, c, sl * TT:(sl + 1) * TT], pb[:, :], [Bpb], [BkTc[sl][c]]))
                    for c in range(3):
                        proj_fm(1792 + c * 128, lambda pb, Bpb: evac(kT_s[:, c, t * TT:(t + 1) * TT], pb[:, :], [Bpb], [BkTs[t][c]]))
                    if t == 1:
                        chk("t1b")
                    if own:
                        for c in range(3):
                            proj_fm(256 + c * 128, lambda pb, Bpb: evac(qT_c[:, c, :], pb[:, :], [Bpb], [BqTc[c]], scale=0.125))
                        for c in range(3):
                            proj_fm(1408 + c * 128, lambda pb, Bpb: evac(qT_s[:, c, :], pb[:, :], [Bpb], [BqTs[c]], scale=0.125))
                    if t == 1:
                        chk("t1c")
                    for s in range(4):
                        for (col, dst, Bd) in ((1024, v_c[:, sl * 4 + s, :], Bvc[sl][s]), (2176, v_s[:, t * 4 + s, :], Bvs[t][s])):
                            pb, Bpb = pjbank()
                            wb = sorted(set([col // 640, (col + 383) // 640]))
                            for k in range(8):
                                S.op("pe", lambda e: e.matmul(pb[:, 0:384], lhsT=xb[:, k, s * 128:(s + 1) * 128], rhs=w_in_bf[:, k, col:col + 384],
                                                              start=(k == 0), stop=(k == 7)),
                                     [Bwin[i] for i in wb] + [Bxb], [Bpb], signal=(k == 7))
                            evac(dst, pb[:, 0:384], [Bpb], [Bd])
                    if t + 1 < NT:
                        S.dma("pool", xb[:], xsrc_v[:, :, (t + 1) * TT:(t + 2) * TT], Bxb, reads=[Bxsrc[t + 1]], writes=[Bxb])
                    if not own:
                        continue

                    chk("proj1")
                    W = 16 + TT
                    (TA, BTA), (TB, BTB) = zp.get(), zp.get()
                    Bpt = [BTA, BTB]
                    pooled = []
                    for c in range(2):
                        pl, Bpl = bp.get()
                        pooled.append((pl, Bpl))

                        def pool_out(p0, Tsrc, BT, inv):
                            S.op("dve", lambda e: e.scalar_tensor_tensor(out=pl[p0:p0 + 64, :], in0=Tsrc[p0:p0 + 64, 16:W], scalar=inv,
                                                                         in1=U[p0:p0 + 64, c, 16:W], op0=ALU.mult, op1=ALU.subtract),
                                 [BT, Bu[sl]], [Bpl])
                            if first_own:
                                tmp, Btmp = fp.get()
                                S.op("dve", lambda e: e.tensor_tensor(out=tmp[p0:p0 + 64, 0:16], in0=Tsrc[p0:p0 + 64, 16:32],
                                                                      in1=prm[p0:p0 + 64, P_ICNT + c * 16:P_ICNT + c * 16 + 16], op=ALU.mult),
                                     [BT, Bprm], [Btmp])
                                S.op("dve", lambda e: e.tensor_tensor(out=pl[p0:p0 + 64, 0:16], in0=tmp[p0:p0 + 64, 0:16],
                                                                      in1=U[p0:p0 + 64, c, 16:32], op=ALU.subtract),
                                     [Btmp, Bu[sl]], [Bpl])

                        S.op("dve", lambda e: e.tensor_tensor(out=TA[:, 1:W], in0=U[:, c, 1:W], in1=U[:, c, 0:W - 1], op=ALU.add), [Bu[sl]], [Bpt[0]])
                        S.op("dve", lambda e: e.tensor_tensor(out=TB[:, 3:W], in0=TA[:, 3:W], in1=TA[:, 1:W - 2], op=ALU.add), [Bpt[0]], [Bpt[1]])
                        if c == 0:
                            pool_out(0, TA, Bpt[0], 0.5)
                            pool_out(64, TB, Bpt[1], 0.25)
                        else:
                            S.op("dve", lambda e: e.tensor_tensor(out=TA[:, 7:W], in0=TB[:, 7:W], in1=TB[:, 3:W - 4], op=ALU.add), [Bpt[1]], [Bpt[0]])
                            pool_out(0, TA, Bpt[0], 0.125)
                            S.op("dve", lambda e: e.tensor_tensor(out=TB[:, 15:W], in0=TA[:, 15:W], in1=TA[:, 7:W - 8], op=ALU.add), [Bpt[0]], [Bpt[1]])
                            pool_out(64, TB, Bpt[1], 0.0625)
                    Yp = []
                    for c in range(2):
                        pb, Bpb = pjbank()
                        S.op("pe", lambda e: e.matmul(pb[:, :], lhsT=pw_bf[:, c, :], rhs=pooled[c][0][:, :], start=True, stop=True),
                             [Bpw, pooled[c][1]], [Bpb])
                        y, By = ytile[c], Bytile[c]
                        S.op("act", lambda e: e.activation(out=y[:], in_=pb[:, :], func=AF.Identity, scale=pcol(P_PSCALE + c)),
                             [Bpb, Bprm], [By])
                        Yp.append((y, By))

                    def rms_group(Ys, nfeat, ci0):
                        srcs = []
                        for (y, By) in Ys:
                            sq, Bsq = fp.get()
                            S.op("pool", lambda e: e.tensor_tensor(out=sq[:], in0=y[:], in1=y[:], op=ALU.mult), [By], [Bsq])
                            srcs.append((sq[:], Bsq))
                        r, Br = stats_rinv(srcs, 1.0 / nfeat, RMS_EPS, fp, STb, BST)
                        for i, (y, By) in enumerate(Ys):
                            ci = ci0 + i
                            S.op("dve", lambda e: e.scalar_tensor_tensor(out=yn[:, ci, :], in0=y[:], scalar=pcol(P_GMIX + ci), in1=r[:],
                                                                         op0=ALU.mult, op1=ALU.mult), [By, Br, Bprm], [Byn[ci]])

                    rms_group(Yp, 256, 0)

                    chk("pool1")
                    Yc = []
                    Rbank = ps[4]
                    for hp in range(3):
                        jlist = [j_ for j_ in (3, 4, 2, 5, 1, 6, 0, 7) if (t > 0 or j_ >= 4)]
                        units = [(hh, j) for j in jlist for hh in range(2)]
                        cu = {}
                        firstC = [True, True]

                        def ca(u):
                            hh, j = units[u]
                            h = 2 * hp + hh
                            hb = hh * 64
                            jsl = sl if j >= 4 else 1 - sl
                            kcol = jsl * TT + (j % 4) * 128
                            qc_lo = max(0, 2 * j - 8)
                            qc_hi = min(7, 2 * j + 1)
                            qlo, qhi = qc_lo * 64, qc_hi * 64 + 64
                            N = qhi - qlo
                            Dd = qlo + 512 - 128 * j
                            masked = ((not full) and t == 1 and j < 4)
                            zb, Bzb = Z[u % 2], BZ[u % 2]
                            S.op("pe", lambda e: e.matmul(zb[:, 0:N], lhsT=kT_c[hb:hb + 64, hp, kcol:kcol + 128],
                                                          rhs=qT_c[hb:hb + 64, hp, qlo:qhi], start=True, stop=True),
                                 [BkTc[jsl][hp], BqTc[hp]], [Bzb])
                            if j <= 2:
                                ntab = 0
                            elif j == 3:
                                ntab = 128
                            else:
                                ntab = min(256, N)
                            P, BP = bp.get()
                            if ntab > 0:
                                tt_, Btt = fp.get()
                                g0 = P_GTAB + h * 256 + Dd
                                S.op("dve", lambda e: e.tensor_tensor(out=tt_[:, 0:ntab], in0=zb[:, 0:ntab], in1=prm[:, g0:g0 + ntab], op=ALU.add),
                                     [Bzb, Bprm], [Btt])
                                if masked:
                                    S.op("act", lambda e: e.activation(out=P[:, 0:ntab], in_=tt_[:, 0:ntab], func=AF.Exp, bias=pcol(P_CMASK + 1)),
                                         [Btt, Bprm], [BP])
                                else:
                                    S.op("act", lambda e: e.activation(out=P[:, 0:ntab], in_=tt_[:, 0:ntab], func=AF.Exp), [Btt], [BP])
                            if N > ntab:
                                bias_ap = small[:, h:h + 1] if masked else pcol(P_BIASC + h)
                                S.op("act", lambda e: e.activation(out=P[:, ntab:N], in_=zb[:, ntab:N], func=AF.Exp, bias=bias_ap),
                                     [Bzb, Bprm, Bsmall], [BP])
                            if j <= 3:
                                c0 = (2 * j + 1) * 64 - qlo
                                S.op("pool", lambda e: e.memset(P[0:64, c0:c0 + 64], 0.0), [], [BP])
                            else:
                                S.op("pool", lambda e: e.memset(P[64:128, 0:64], 0.0), [], [BP])
                            cu[u] = (hh, j, h, hb, jsl, qlo, qhi, N, P, BP)

                        def cb_(u):
                            hh, j, h, hb, jsl, qlo, qhi, N, P, BP = cu.pop(u)
                            S.op("pe", lambda e: e.matmul(Obank[hb:hb + 64, qlo:qhi], lhsT=v_c[:, jsl * 4 + j % 4, h * 64:(h + 1) * 64], rhs=P[:, 0:N],
                                                          start=firstC[hh], stop=(j == 7), skip_group_check=True),
                                 [Bvc[jsl][j % 4], BP], [BO[hh]])
                            S.op("pe", lambda e: e.matmul(Rbank[hb:hb + 64, qlo:qhi], lhsT=cstb[:, C_ONES:C_ONES + 64], rhs=P[:, 0:N],
                                                          start=firstC[hh], stop=(j == 7), skip_group_check=True),
                                 [Bcstb, BP], [BR2[hh]])
                            firstC[hh] = False

                        nu = len(units)
                        ca(0)
                        if nu > 1:
                            ca(1)
                        for u in range(nu):
                            cb_(u)
                            if u + 2 < nu:
                                ca(u + 2)
                                S.op("pe", lambda e: e.matmul(ps[0][:, 0:128], lhsT=cstb[:, C_ONES:C_ONES + 128], rhs=cstb[:, 0:128],
                                                              start=True, stop=True), [Bcstb], [Bps[0]], signal=False)
                        rec, Brec = fp.get()
                        S.op("dve", lambda e: e.reciprocal(out=rec[:], in_=Rbank[:, :]), BR2, [Brec])
                        y, By = ytile[hp], Bytile[hp]
                        S.op("dve", lambda e: e.tensor_tensor(out=y[:], in0=Obank[:, :], in1=rec[:], op=ALU.mult), BO + [Brec], [By])
                        Yc.append((y, By))
                    rms_group(Yc, 384, 2)

                    chk("chunk1")
                    Ys = []
                    nkt = 4 * t + 4
                    for hp in range(3):
                        tasks = [(hh, kt) for kt in range(nkt - 1, -1, -1) for hh in range(2)]
                        st = {}
                        firstA = [True, True]
                        firstO = [True, True]

                        def s1(i):
                            hh, kt = tasks[i]
                            hb = hh * 64
                            r = kt - 4 * t
                            c0 = 128 * r if r >= 0 else 0
                            N = TT - c0
                            masked = (not full) and kt < 4
                            zb, Bzb = Z[i % 2], BZ[i % 2]
                            S.op("pe", lambda e: e.matmul(zb[:, 0:N], lhsT=kT_s[hb:hb + 64, hp, kt * 128:(kt + 1) * 128],
                                                          rhs=qT_s[hb:hb + 64, hp, c0:TT], start=True, stop=True),
                                 [BkTs[kt // 4][hp], BqTs[hp]], [Bzb])
                            zs, Bzs = zp.get()
                            S.op("dve", lambda e: e.tensor_copy(out=zs[:, 0:N], in_=zb[:, 0:N]), [Bzb], [Bzs])
                            E, BE = fp.get()
                            S.op("act", lambda e: e.activation(out=E[:, 0:N], in_=zb[:, 0:N], func=AF.Exp), [Bzb], [BE])
                            SP, BSP = bp.get()
                            if masked:
                                S.op("act", lambda e: e.activation(out=SP[:, 0:N], in_=E[:, 0:N], func=AF.Ln, scale=pcol(P_CMASK), bias=1.0),
                                     [BE, Bprm], [BSP])
                            else:
                                S.op("act", lambda e: e.activation(out=SP[:, 0:N], in_=E[:, 0:N], func=AF.Ln, bias=1.0), [BE], [BSP])
                            if r >= 0:
                                S.op("pool", lambda e: e.tensor_tensor(out=SP[:, 0:128], in0=SP[:, 0:128], in1=cstb[:, C_MASK:C_MASK + 128], op=ALU.mult),
                                     [BSP, Bcstb], [BSP])
                            st[i] = dict(hh=hh, kt=kt, hb=hb, r=r, c0=c0, N=N, masked=masked, SP=SP, BSP=BSP, zs=zs, Bzs=Bzs)

                        def s2(i):
                            d = st[i]
                            hh, kt, hb, c0, N = d["hh"], d["kt"], d["hb"], d["c0"], d["N"]
                            zs, Bzs = d["zs"], d["Bzs"]
                            Ab, BAb = Abank[hh], BA[hh]
                            S.op("pe", lambda e: e.matmul(Ab[:, c0:TT], lhsT=cstb[:, C_TRI:C_TRI + 128], rhs=d["SP"][:, 0:N],
                                                          start=firstA[hh], stop=True, skip_group_check=True),
                                 [Bcstb, d["BSP"]], BAb)
                            firstA[hh] = False
                            S.op("dve", lambda e: e.tensor_tensor(out=zs[:, 0:N], in0=Ab[:, c0:TT], in1=zs[:, 0:N], op=ALU.subtract),
                                 BAb + [Bzs], [Bzs])
                            Wt, BW = bp.get()
                            if d["masked"]:
                                S.op("act", lambda e: e.activation(out=Wt[:, 0:N], in_=zs[:, 0:N], func=AF.Exp, scale=-1.0, bias=pcol(P_CMASK + 1)),
                                     [Bzs, Bprm], [BW])
                            else:
                                S.op("act", lambda e: e.activation(out=Wt[:, 0:N], in_=zs[:, 0:N], func=AF.Exp, scale=-1.0), [Bzs], [BW])
                            if d["r"] >= 0:
                                S.op("pool", lambda e: e.tensor_tensor(out=Wt[:, 0:128], in0=Wt[:, 0:128], in1=cstb[:, C_MASK:C_MASK + 128], op=ALU.mult),
                                     [BW, Bcstb], [BW])
                            d["W"], d["BW"] = Wt, BW

                        def filler(nf_):
                            for _ in range(nf_):
                                S.op("pe", lambda e: e.matmul(ps[0][:, 0:256], lhsT=cstb[:, C_ONES:C_ONES + 128], rhs=cstb[:, 0:256],
                                                              start=True, stop=True), [Bcstb], [Bps[0]], signal=False)

                        def s2b(i):
                            d = st[i]
                            hh, kt, c0, N = d["hh"], d["kt"], d["c0"], d["N"]
                            Ab, BAb = Abank[hh], BA[hh]
                            filler(NFILL)
                            if kt > 0:
                                S.op("pe", lambda e: e.matmul(Ab[:, c0:TT], lhsT=cstb[:, C_TRIU:C_TRIU + 128], rhs=d["SP"][:, 0:N],
                                                              start=False, stop=True, skip_group_check=True),
                                     [Bcstb, d["BSP"]], BAb)

                        def s3(i):
                            d = st.pop(i)
                            hh, kt, hb, c0, N = d["hh"], d["kt"], d["hb"], d["c0"], d["N"]
                            h = 2 * hp + hh
                            last = (kt == 0)
                            S.op("pe", lambda e: e.matmul(Obank[hb:hb + 64, c0:TT], lhsT=v_s[:, kt, h * 64:(h + 1) * 64], rhs=d["W"][:, 0:N],
                                                          start=firstO[hh], stop=last, skip_group_check=True),
                                 [Bvs[kt // 4][kt % 4], d["BW"]], [BO[hh]])
                            firstO[hh] = False
                            filler(NFILL)

                        n = len(tasks)
                        for i in range(-2, n + 2):
                            if 0 <= i < n:
                                s2(i)
                            if 0 <= i + 2 < n:
                                s1(i + 2)
                            if 0 <= i - 1 < n:
                                s2b(i - 1)
                            if 0 <= i - 2 < n:
                                s3(i - 2)
                        y, By = ytile[hp], Bytile[hp]
                        S.op("dve", lambda e: e.tensor_copy(out=y[:], in_=Obank[:, :]), BO, [By])
                        Ys.append((y, By))
                    rms_group(Ys, 384, 5)

                    if debug:
                        Bd = Buf()
                        S.dma("sp", dbg_yn[:, :, own_i * TT:(own_i + 1) * TT], yn[:], Bd, reads=Byn, writes=[Bd])

                    chk("sb1")
                    for dch in range(8):
                        pb, Bpb = pjbank()
                        for k in range(8):
                            S.op("pe", lambda e: e.matmul(pb[:, :], lhsT=w_out_bf[:, k, dch * 128:(dch + 1) * 128], rhs=yn[:, k, :],
                                                          start=(k == 0), stop=(k == 7)),
                                 [Bwout, Byn[k]], [Bpb], signal=(k == 7))
                        S.op("dve", lambda e: e.scalar_tensor_tensor(out=xres[:, dch, :], in0=xres[:, dch, :], scalar=ALPHA, in1=pb[:, :],
                                                                     op0=ALU.mult, op1=ALU.add), [Bxres[dch], Bxres_d, Bpb], [Bxres[dch]])
                    layer_norm(lambda d_: xres[:, d_, :], Bxres, P_LN1G, P_LN1B, fp, STb, BST, ps[2], Bps[2], lnMR, BlnMR)
                    S.dma("sp", x1s_v[:, :, own_i * TT:(own_i + 1) * TT], xres[:], Bxres_d, reads=Bxres + [Bxres_d], writes=[Bx1s[own_i]])
                    own_i += 1
                chk("mixer")
                S.barrier()
                for e_ in ENGS:
                    for b in Bx1s:
                        if not S.dead:
                            S._wait(e_, b.w)

            for hf in range(2 if full else 1):
                pfx2 = "L%dh%d_" % (li, hf)
                cb = hf * 4 * TT
                with ExitStack() as es:
                    sb = lambda name, shape, dtp: es.enter_context(nc.sbuf_tensor(pfx2 + name, shape, dtp))
                    x1bf = sb("x1bf", [128, 8, 4 * TT], BF16)
                    acc = sb("acc", [128, 8, 4 * TT], F32)
                    NWS = 2
                    wg_bf = [sb("wg_bf%d" % i, [128, 8, 512], BF16) for i in range(NWS)]
                    wu_bf = [sb("wu_bf%d" % i, [128, 8, 512], BF16) for i in range(NWS)]
                    wd_bf = [sb("wd_bf%d" % i, [128, 4, D], BF16) for i in range(NWS)]
                    hact = [sb("hact%d" % i, [128, 4, TT], BF16) for i in range(2)]
                    fp = TPool(nc, es, pfx2 + "ffp", [128, 512], F32, 6)
                    lnMR = [sb("flnMR%d" % i, [128, TT], F32) for i in range(2)]
                    BlnMR = [Buf(), Buf()]
                    Bx1bf = [Buf() for i in range(4)]
                    Bacc = [[Buf() for i in range(4)] for d_ in range(8)]
                    Bwg = [Buf() for i in range(NWS)]
                    Bwu = [Buf() for i in range(NWS)]
                    Bwd = [Buf() for i in range(NWS)]
                    Bhact = [[Buf() for c in range(4)] for i in range(2)]
                    def load_x1bf(i):
                        S.dma("pool", x1bf[:, :, i * TT:(i + 1) * TT], x1s_v[:, :, cb + i * TT:cb + (i + 1) * TT], Bx1bf[i], reads=[Bx1s[4 * hf + i]], writes=[Bx1bf[i]])
                    load_x1bf(0)

                    if moe:
                        gB = [sb("gB%d" % i, [128, 4 * TT], F32) for i in range(2)]
                        BgB = [[Buf() for i in range(4)] for _ in range(2)]
                        gates = sb("gates", [128, 16, NE], F32)
                        Bgates = Buf("gates")
                        rt = sb("rt", [128, 8, NE], F32)
                        Brt = Buf("rt")
                        x1f = [sb("x1f0", [128, 8, 128], F32)] * 2
                        Bx1f = [Buf()] * 2
                        gsm = sb("gsm", [128, 64], F32)
                        Bgsm = Buf()
                        gexp = [sb("gexp%d" % i, [128, 128], F32) for i in range(2)]
                        Bgexp = [Buf(), Buf()]
                        S.dma("sp", rt[:], router.rearrange("(k p) e -> p k e", p=128), Brt, writes=[Brt])
                        RB, BRB = ps[7], Bps[7]
                        for s in range(16):
                            xf, Bxf = x1f[s % 2], Bx1f[s % 2]
                            S.dma("sp", xf[:], x1s_v[:, :, cb + s * 128:cb + (s + 1) * 128], Bxf, reads=[Bx1s[4 * hf + s // 4]], writes=[Bxf])
                            for k in range(8):
                                S.op("pe", lambda e: e.matmul(RB[:, 0:NE], lhsT=xf[:, k, :], rhs=rt[:, k, :], start=(k == 0), stop=(k == 7)),
                                     [Bxf, Brt], [BRB])
                            lg = gsm[:, 0:8]
                            S.op("act", lambda e: e.activation(out=lg, in_=RB[:, 0:NE], func=AF.Copy), [BRB], [Bgsm])
                            m8 = gsm[:, 8:16]
                            S.op("dve", lambda e: e.max(out=m8, in_=lg), [Bgsm], [Bgsm])
                            dd = gsm[:, 16:17]
                            S.op("dve", lambda e: e.tensor_tensor(out=dd, in0=gsm[:, 9:10], in1=gsm[:, 8:9], op=ALU.subtract), [Bgsm], [Bgsm])
                            ee = gsm[:, 17:18]
                            S.op("act", lambda e: e.activation(out=ee, in_=dd, func=AF.Exp), [Bgsm], [Bgsm])
                            den = gsm[:, 18:19]
                            S.op("dve", lambda e: e.tensor_scalar(out=den, in0=ee, scalar1=1.0, scalar2=None, op0=ALU.add), [Bgsm], [Bgsm])
                            w1 = gsm[:, 19:20]
                            S.op("dve", lambda e: e.reciprocal(out=w1, in_=den), [Bgsm], [Bgsm])
                            w2 = gsm[:, 20:21]
                            S.op("dve", lambda e: e.tensor_tensor(out=w2, in0=ee, in1=w1, op=ALU.mult), [Bgsm], [Bgsm])
                            m1 = gsm[:, 24:32]
                            S.op("dve", lambda e: e.tensor_scalar(out=m1, in0=lg, scalar1=gsm[:, 8:9], scalar2=w1, op0=ALU.is_equal, op1=ALU.mult),
                                 [Bgsm], [Bgsm])
                            m2 = gsm[:, 32:40]
                            S.op("dve", lambda e: e.tensor_scalar(out=m2, in0=lg, scalar1=gsm[:, 9:10], scalar2=w2, op0=ALU.is_equal, op1=ALU.mult),
                                 [Bgsm], [Bgsm])
                            S.op("dve", lambda e: e.tensor_tensor(out=gates[:, s, :], in0=m1, in1=m2, op=ALU.add), [Bgsm], [Bgates])

                    GU = [(ps[0], Bps[0], ps[1], Bps[1]), (ps[2], Bps[2], ps[3], Bps[3])]
                    OBk = [(ps[4], Bps[4]), (ps[5], Bps[5]), (ps[6], Bps[6])]
                    groups = [(0, 4), (4, 4), (8, 4), (12, 4), (16, 4), (20, 2)]
                    ws_i = 0
                    gu_i = 0
                    ob_i = 0
                    ha_i = 0
                    first_acc = True
                    def emit_gb(ex_):
                        gb_, Bgb_ = gB[ex_ % 2], BgB[ex_ % 2]
                        for s in range(16):
                            ge, Bge = gexp[s % 2], Bgexp[s % 2]
                            S.op("dve", lambda e: e.tensor_scalar(out=ge[:], in0=onesf, scalar1=gates[:, s, ex_:ex_ + 1], scalar2=None, op0=ALU.mult),
                                 [Bcstf, Bgates], [Bge])
                            S.op("pe", lambda e: e.matmul(ps[7][:, (s % 4) * 128:(s % 4 + 1) * 128], lhsT=ge[:], rhs=identf, start=True, stop=True),
                                 [Bge, Bcstf], [Bps[7]])
                            if s % 4 == 3:
                                i4 = s // 4
                                S.op("act", lambda e: e.activation(out=gb_[:, i4 * TT:(i4 + 1) * TT], in_=ps[7][:, :], func=AF.Copy), [Bps[7]], [Bgb_[i4]])

                    if moe:
                        emit_gb(0)
                    for ex in range(NE if moe else 1):
                        if moe:
                            gb, Bgb = gB[ex % 2], BgB[ex % 2]
                        for gi_, (f0, nf) in enumerate(groups):
                            if moe and gi_ == 3 and ex + 1 < NE:
                                emit_gb(ex + 1)
                            w_i = ws_i % NWS
                            ws_i += 1
                            S.dma("pool", wg_bf[w_i][:, :, 0:nf * 128], wg[ex, 131072 * f0:131072 * (f0 + nf)].rearrange("(p k f) -> p k f", p=128, k=8),
                                  Bwg[w_i], writes=[Bwg[w_i]])
                            S.dma("pool", wu_bf[w_i][:, :, 0:nf * 128], wu[ex, 131072 * f0:131072 * (f0 + nf)].rearrange("(p k f) -> p k f", p=128, k=8),
                                  Bwu[w_i], writes=[Bwu[w_i]])
                            S.dma("pool", wd_bf[w_i][:, 0:nf, :], wd[ex, 131072 * f0:131072 * (f0 + nf)].rearrange("(p c d) -> p c d", p=128, c=nf),
                                  Bwd[w_i], writes=[Bwd[w_i]])
                            if ex == 0 and gi_ == 0:
                                for i_ in range(1, 4):
                                    load_x1bf(i_)
                            for i in range(4):
                                ha, Bha = hact[ha_i % 2], Bhact[ha_i % 2]
                                ha_i += 1
                                for fc in range(nf):
                                    G, BG, Ub, BU = GU[gu_i % 2]
                                    gu_i += 1
                                    for k in range(8):
                                        S.op("pe", lambda e: e.matmul(G[:, :], lhsT=wg_bf[w_i][:, k, fc * 128:(fc + 1) * 128], rhs=x1bf[:, k, i * TT:(i + 1) * TT],
                                                                      start=(k == 0), stop=(k == 7)), [Bwg[w_i], Bx1bf[i]], [BG], signal=(k == 7))
                                    for k in range(8):
                                        S.op("pe", lambda e: e.matmul(Ub[:, :], lhsT=wu_bf[w_i][:, k, fc * 128:(fc + 1) * 128], rhs=x1bf[:, k, i * TT:(i + 1) * TT],
                                                                      start=(k == 0), stop=(k == 7)), [Bwu[w_i], Bx1bf[i]], [BU], signal=(k == 7))
                                    sg, Bsg = fp.get()
                                    S.op("act", lambda e: e.activation(out=sg[:], in_=G[:, :], func=AF.Silu), [BG], [Bsg])
                                    if moe:
                                        S.op("dve", lambda e: e.tensor_tensor(out=sg[:], in0=sg[:], in1=Ub[:, :], op=ALU.mult), [Bsg, BU], [Bsg])
                                        S.op("dve", lambda e: e.tensor_tensor(out=ha[:, fc, :], in0=sg[:], in1=gb[:, i * TT:(i + 1) * TT], op=ALU.mult),
                                             [Bsg, Bgb[i]], [Bha[fc]])
                                    else:
                                        S.op("dve", lambda e: e.tensor_tensor(out=ha[:, fc, :], in0=sg[:], in1=Ub[:, :], op=ALU.mult), [Bsg, BU], [Bha[fc]])
                                for dch in range(8):
                                    ob, Bob = OBk[ob_i % 3]
                                    ob_i += 1
                                    for fc in range(nf):
                                        S.op("pe", lambda e: e.matmul(ob[:, :], lhsT=wd_bf[w_i][:, fc, dch * 128:(dch + 1) * 128], rhs=ha[:, fc, :],
                                                                      start=(fc == 0), stop=(fc == nf - 1)), [Bwd[w_i], Bha[fc]], [Bob], signal=(fc == nf - 1))
                                    if first_acc:
                                        S.op("act", lambda e: e.activation(out=acc[:, dch, i * TT:(i + 1) * TT], in_=ob[:, :], func=AF.Copy), [Bob], [Bacc[dch][i]])
                                    else:
                                        S.op("dve", lambda e: e.tensor_tensor(out=acc[:, dch, i * TT:(i + 1) * TT], in0=acc[:, dch, i * TT:(i + 1) * TT],
                                                                              in1=ob[:, :], op=ALU.add), [Bacc[dch][i], Bob], [Bacc[dch][i]])
                            first_acc = False

                    for i in range(4):
                        for dch in range(8):
                            xt_, Bxt = fp.get()
                            S.dma("sp", xt_[:], x1s[dch * 128:(dch + 1) * 128, cb + i * TT:cb + (i + 1) * TT], Bxt, reads=[Bx1s[4 * hf + i]], writes=[Bxt])
                            S.op("dve", lambda e: e.scalar_tensor_tensor(out=acc[:, dch, i * TT:(i + 1) * TT], in0=xt_[:], scalar=ALPHA,
                                                                         in1=acc[:, dch, i * TT:(i + 1) * TT], op0=ALU.mult, op1=ALU.add),
                                 [Bxt, Bacc[dch][i]], [Bacc[dch][i]])
                        Bv = [Bacc[d_][i] for d_ in range(8)]
                        layer_norm(lambda d_: acc[:, d_, i * TT:(i + 1) * TT], Bv, P_LN2G, P_LN2B, fp, ps[7], Bps[7], ps[6], Bps[6], lnMR, BlnMR)
                        Bo_sb = Buf()
                        S.dma("sp", dst_v[:, :, cb + i * TT:cb + (i + 1) * TT], acc[:, :, i * TT:(i + 1) * TT], Bo_sb, reads=Bv, writes=[Bdst[4 * hf + i]])
                S.barrier()
                for e_ in ENGS:
                    for b in Bdst[4 * hf:4 * hf + 4]:
                        if not S.dead:
                            S._wait(e_, b.w)

        Bxin = [Buf() for t in range(NT)]
        Bx2s = [Buf("x2s%d" % t) for t in range(NT)]
        Bxs2 = [Buf("xs2%d" % t) for t in range(NT)]
        emit_layer(0, False, True, xT_v, Bxin, x2s_v, Bx2s)
        with ExitStack() as es:
            S.dma("sp", prm[:], prm_all[1], Bprm, writes=[Bprm])
            bl = TPool(nc, es, "bl", [128, 8, TT], F32, 4)
            for t in range(NT):
                a1, Ba1 = bl.get()
                S.dma("sp", a1[:], x2s_v[:, :, t * TT:(t + 1) * TT], Ba1, reads=[Bx2s[t]], writes=[Ba1])
                S.op("dve", lambda e: e.tensor_scalar(out=a1[:], in0=a1[:], scalar1=pcol(P_CMASK), scalar2=None, op0=ALU.mult), [Ba1, Bprm], [Ba1])
                if t > 0:
                    a0, Ba0 = bl.get()
                    S.dma("sp", a0[:], x2s_v[:, :, (t - 1) * TT:t * TT], Ba0, reads=[Bx2s[t - 1]], writes=[Ba0])
                    S.op("dve", lambda e: e.scalar_tensor_tensor(out=a1[:], in0=a0[:], scalar=pcol(P_M0), in1=a1[:], op0=ALU.mult, op1=ALU.add),
                         [Ba0, Ba1, Bprm], [Ba1])
                S.dma("sp", xs2_v[:, :, t * TT:(t + 1) * TT], a1[:], Ba1, reads=[Ba1], writes=[Bxs2[t]])
            S.barrier()
            for e_ in ENGS:
                for b in Bxs2:
                    S._wait(e_, b.w)
        emit_layer(1, True, False, xs2_v, Bxs2, outT_v, Bout + [Buf() for _ in range(4)])
        S.finish(Bout)
        print("built fused: ops=%d waits=%d dma_sems=%d" % (S.nop, S.nwait, S.ndsem))
    return nc


def _consts():
    c = np.zeros((128, NCST), np.float32)
    j = np.arange(128)[:, None]
    k = np.arange(128)[None, :]
    c[:, C_TRI:C_TRI + 128] = (j >= k)
    c[:, C_TRIU:C_TRIU + 128] = (j < k)
    c[:, C_ONES:C_ONES + 128] = 1.0
    c[:, C_MASK:C_MASK + 128] = (j < k)
    c[:, C_IDENT:C_IDENT + 128] = np.eye(128)
    return c


def _cols(v):
    return np.ascontiguousarray(v.reshape(8, 128).T)


def _prm(j, li, full, pool_scale, g_mix, ln1_g, ln1_b, ln2_g, ln2_b, rel_bias):
    p = np.zeros((128, NPRM), np.float32)
    p[:, P_PSCALE:P_PSCALE + 2] = pool_scale[li].reshape(2, 128).T
    p[:, P_GMIX:P_GMIX + 8] = _cols(g_mix[li])
    p[:, P_LN1G:P_LN1G + 8] = _cols(ln1_g[li])
    p[:, P_LN1B:P_LN1B + 8] = _cols(ln1_b[li])
    p[:, P_LN2G:P_LN2G + 8] = _cols(ln2_g[li])
    p[:, P_LN2B:P_LN2B + 8] = _cols(ln2_b[li])
    rb = rel_bias[li]
    p[:, P_BIASC:P_BIASC + 6] = rb[:, 256][None, :]
    real = full or j == 1
    p[:, P_CMASK] = 1.0 if real else 0.0
    p[:, P_CMASK + 1] = 0.0 if real else -100.0
    p[:, P_M0] = 0.0 if j == 1 else 1.0
    wins = np.array([2, 4, 8, 16], np.float32)
    tau = np.arange(16, dtype=np.float32)
    seq_start = full or j == 0
    for c in range(2):
        for half in range(2):
            w = wins[c * 2 + half]
            if seq_start:
                ic = 1.0 / np.minimum(tau + 1.0, w)
            else:
                ic = np.full(16, 1.0 / w, np.float32)
            p[half * 64:(half + 1) * 64, P_ICNT + c * 16:P_ICNT + c * 16 + 16] = ic[None, :]
    pp = np.arange(128)[:, None]
    uu = np.arange(256)[None, :]
    idx = np.clip(uu - pp, -128, 128) + 128
    for h in range(6):
        p[:, P_GTAB + h * 256:P_GTAB + (h + 1) * 256] = rb[h][idx]
    return p


_GROUPS = [(0, 4), (4, 4), (8, 4), (12, 4), (16, 4), (20, 2)]


def _relayout_gu(w):
    E = w.shape[0]
    out = np.empty((E, D * DFF), np.float32)
    for e in range(E):
        w3 = w[e].reshape(8, 128, DFF)
        pos = 0
        for (f0, nf) in _GROUPS:
            blk = w3[:, :, f0 * 128:(f0 + nf) * 128].transpose(1, 0, 2)
            out[e, pos:pos + blk.size] = blk.reshape(-1)
            pos += blk.size
    return out


def _relayout_d(w):
    E = w.shape[0]
    out = np.empty((E, DFF * D), np.float32)
    for e in range(E):
        pos = 0
        for (f0, nf) in _GROUPS:
            blk = w[e][f0 * 128:(f0 + nf) * 128].reshape(nf, 128, D).transpose(1, 0, 2)
            out[e, pos:pos + blk.size] = blk.reshape(-1)
            pos += blk.size
    return out


_NC = []


def kernel(**inputs):
    inp = {k: np.asarray(v) for k, v in inputs.items()}
    x = np.ascontiguousarray(inp["x"], dtype=np.float32)
    if not _NC:
        _NC.append(build_fused())
    nc = _NC[0]
    cst = _consts()
    pwbd = np.zeros((2, 128, 2, 128), np.float32)
    for li in range(2):
        for c in range(2):
            pwbd[li, 0:64, c, 0:64] = inp["pool_w"][li][2 * c]
            pwbd[li, 64:128, c, 64:128] = inp["pool_w"][li][2 * c + 1]
    wg_r, wu_r, wd_r = _relayout_gu(inp["ffn_wg"]), _relayout_gu(inp["ffn_wu"]), _relayout_d(inp["ffn_wd"])
    mwg_r, mwu_r, mwd_r = _relayout_gu(inp["moe_wg"][0]), _relayout_gu(inp["moe_wu"][0]), _relayout_d(inp["moe_wd"][0])
    in_maps = []
    for core in range(8):
        b, j = core // 2, core % 2
        prm = np.stack([_prm(j, li, li == 0, inp["pool_scale"], inp["g_mix"], inp["ln1_g"], inp["ln1_b"], inp["ln2_g"],
                             inp["ln2_b"], inp["rel_bias"]) for li in range(2)])
        in_maps.append({"xT": np.ascontiguousarray(x[b].T), "w_in": inp["w_in"], "w_out": inp["w_out"], "pwbd": pwbd,
                        "prm": prm, "cst": cst, "wg": wg_r, "wu": wu_r, "wd": wd_r, "mwg": mwg_r, "mwu": mwu_r, "mwd": mwd_r,
                        "router": inp["moe_router"][0]})
    res = run_bass_kernel_spmd(nc, in_maps, core_ids=list(range(8)))
    out = np.empty_like(x)
    for core in range(8):
        b, j = core // 2, core % 2
        o = res.results[core]["outT"]
        for i in range(4):
            g = 2 * i + j
            out[b, g * TT:(g + 1) * TT, :] = o[:, i * TT:(i + 1) * TT].T
    return out
```
